# Optimizing a Trainium2 kernel written in Bass

```python
import math
import jax
import jax.numpy as jnp
from jax import lax
import numpy as np

D_MODEL = 1024
BATCH = 2
SEQ = 16384
DEPTH = 2

GRID_W = 64
CTX_LEN = 256
HEAD_DIM = 64
N_GROUP_HEADS = 4
NA_WIN_H = 8
NA_WIN_W = 16
MLA_Q_RANK = 256
MLA_KV_RANK = 128
MLA_NOPE_DIM = 64
MLA_ROPE_DIM = 32
MLA_V_DIM = 64
DIFF_QK_DIM = 32
DIFF_V_DIM = 64
GQA_KV_HEADS = 2
N_EXPERTS = 16
EC_CAPACITY = 2
D_FF_EXPERT = 2816
ROPE_THETA = 10000.0
NORM_EPS = 1e-6
Q_BLOCK = 128
N_MOD = 6

IN_SPLITS = (
    N_GROUP_HEADS * HEAD_DIM, N_GROUP_HEADS * HEAD_DIM, N_GROUP_HEADS * HEAD_DIM,
    MLA_Q_RANK, MLA_KV_RANK, MLA_ROPE_DIM,
    N_GROUP_HEADS * 2 * DIFF_QK_DIM, N_GROUP_HEADS * 2 * DIFF_QK_DIM, N_GROUP_HEADS * DIFF_V_DIM,
    N_GROUP_HEADS * HEAD_DIM, GQA_KV_HEADS * HEAD_DIM, GQA_KV_HEADS * HEAD_DIM,
)
D_IN = sum(IN_SPLITS)

kernel_name = 'hybrid_parallel_heads_ec_moe_diffusion'


def rms_norm(x, g):
    xf = x.astype(jnp.float32)
    y = xf * lax.rsqrt(jnp.mean(xf * xf, axis=-1, keepdims=True) + NORM_EPS)
    return (y * g.astype(jnp.float32)).astype(x.dtype)


def modulate(x, g, shift, scale):
    return rms_norm(x, g) * (1 + scale) + shift


def split_heads(t, n):
    return t.reshape(t.shape[:-1] + (n, t.shape[-1] // n))


def merge_heads(t):
    return t.reshape(t.shape[:-2] + (-1,))


def split_cols(t):
    pts, acc = [], 0
    for w in IN_SPLITS[:-1]:
        acc += w
        pts.append(acc)
    return jnp.split(t, pts, axis=-1)


def rope_1d(x, pos):
    n = x.shape[-1]
    inv = ROPE_THETA ** (-jnp.arange(0, n, 2, dtype=jnp.float32) / n)
    ang = pos.astype(jnp.float32)[:, None] * inv[None, :]
    bshape = (x.shape[1],) + (1,) * (x.ndim - 3) + (n // 2,)
    cos, sin = jnp.cos(ang).reshape(bshape), jnp.sin(ang).reshape(bshape)
    xf = x.astype(jnp.float32)
    x1, x2 = xf[..., : n // 2], xf[..., n // 2:]
    return jnp.concatenate([x1 * cos - x2 * sin, x1 * sin + x2 * cos], axis=-1).astype(x.dtype)


def rope_2d(x, row, col):
    half = x.shape[-1] // 2
    return jnp.concatenate([rope_1d(x[..., :half], row), rope_1d(x[..., half:], col)], axis=-1)


def attend(q, k, v):
    B, Sq, H, dk = q.shape
    scale = dk ** -0.5
    nb = Sq // Q_BLOCK
    qb = jnp.swapaxes(q.reshape(B, nb, Q_BLOCK, H, dk), 0, 1)

    def one_block(qblk):
        s = jnp.einsum('bqhd,bkhd->bhqk', qblk, k, preferred_element_type=jnp.float32) * scale
        p = jax.nn.softmax(s, axis=-1).astype(v.dtype)
        return jnp.einsum('bhqk,bkhd->bqhd', p, v)

    out = lax.map(one_block, qb)
    return jnp.swapaxes(out, 0, 1).reshape(B, Sq, H, v.shape[-1])


def neighbourhood_attend(q, k, v, kc, vc, rpb, rows):
    B, S, H, d = q.shape
    kh = min(NA_WIN_H, rows)
    n_loc = kh * NA_WIN_W
    scale = d ** -0.5
    qg = q.reshape(B, rows, GRID_W, H, d)
    kg = k.reshape(B, rows, GRID_W, H, d)
    vg = v.reshape(B, rows, GRID_W, H, d)
    cols = jnp.arange(GRID_W)
    col_idx = jnp.clip(cols - NA_WIN_W // 2, 0, GRID_W - NA_WIN_W)[:, None] + jnp.arange(NA_WIN_W)[None, :]
    col_bias_idx = col_idx - cols[:, None] + (NA_WIN_W - 1)

    def one_row(r):
        r0 = jnp.clip(r - kh // 2, 0, rows - kh)
        kr = lax.dynamic_slice_in_dim(kg, r0, kh, axis=1)[:, :, col_idx]
        vr = lax.dynamic_slice_in_dim(vg, r0, kh, axis=1)[:, :, col_idx]
        qr = lax.dynamic_index_in_dim(qg, r, axis=1, keepdims=False)
        row_bias_idx = r0 + jnp.arange(kh) - r + (NA_WIN_H - 1)
        bias = rpb[:, row_bias_idx[None, :, None], col_bias_idx[:, None, :]].astype(jnp.float32)
        s_loc = jnp.einsum('bqhd,bjqwhd->bhqjw', qr, kr, preferred_element_type=jnp.float32) * scale + bias[None]
        s_ctx = jnp.einsum('bqhd,bkhd->bhqk', qr, kc, preferred_element_type=jnp.float32) * scale
        p = jax.nn.softmax(jnp.concatenate([s_loc.reshape(B, H, GRID_W, n_loc), s_ctx], axis=-1), axis=-1).astype(v.dtype)
        p_loc = p[..., :n_loc].reshape(B, H, GRID_W, kh, NA_WIN_W)
        return (jnp.einsum('bhqjw,bjqwhd->bqhd', p_loc, vr)
                + jnp.einsum('bhqk,bkhd->bqhd', p[..., n_loc:], vc))

    out = lax.map(one_row, jnp.arange(rows))
    return jnp.swapaxes(out, 0, 1).reshape(B, S, H, d)


def na_mixer(p, pc, g_q, g_k, rpb, rows, need_ctx):
    q = rms_norm(split_heads(p[0], N_GROUP_HEADS), g_q)
    k = rms_norm(split_heads(p[1], N_GROUP_HEADS), g_k)
    v = split_heads(p[2], N_GROUP_HEADS)
    kc = rms_norm(split_heads(pc[1], N_GROUP_HEADS), g_k)
    vc = split_heads(pc[2], N_GROUP_HEADS)
    o = merge_heads(neighbourhood_attend(q, k, v, kc, vc, rpb, rows))
    oc = None
    if need_ctx:
        qc = rms_norm(split_heads(pc[0], N_GROUP_HEADS), g_q)
        oc = merge_heads(attend(qc, kc, vc))
    return o, oc


def mla_q(p_cq, g_cq, w_uq, g_q, pos):
    q = rms_norm(split_heads(rms_norm(p_cq, g_cq) @ w_uq, N_GROUP_HEADS), g_q)
    if pos is None:
        return q
    return jnp.concatenate([q[..., :MLA_NOPE_DIM], rope_2d(q[..., MLA_NOPE_DIM:], *pos)], axis=-1)


def mla_kv(p_ckv, p_kr, g_ckv, w_ukv, g_kn, g_kr, pos):
    kv = split_heads(rms_norm(p_ckv, g_ckv) @ w_ukv, N_GROUP_HEADS)
    k_nope = rms_norm(kv[..., :MLA_NOPE_DIM], g_kn)
    v = kv[..., MLA_NOPE_DIM:]
    k_rope = rms_norm(p_kr, g_kr)[:, :, None, :]
    if pos is not None:
        k_rope = rope_2d(k_rope, *pos)
    k = jnp.concatenate([k_nope, jnp.broadcast_to(k_rope, k_nope.shape[:-1] + (MLA_ROPE_DIM,))], axis=-1)
    return k, v


def mla_mixer(p, pc, g_cq, w_uq, g_q, g_ckv, w_ukv, g_kn, g_kr, pos, need_ctx):
    q = mla_q(p[0], g_cq, w_uq, g_q, pos)
    k, v = mla_kv(p[1], p[2], g_ckv, w_ukv, g_kn, g_kr, pos)
    kc, vc = mla_kv(pc[1], pc[2], g_ckv, w_ukv, g_kn, g_kr, None)
    o = merge_heads(attend(q, jnp.concatenate([kc, k], axis=1), jnp.concatenate([vc, v], axis=1)))
    oc = None
    if need_ctx:
        qc = mla_q(pc[0], g_cq, w_uq, g_q, None)
        oc = merge_heads(attend(qc, kc, vc))
    return o, oc


def diff_qk(t, g, pos):
    t = rms_norm(t.reshape(t.shape[:-1] + (N_GROUP_HEADS, 2, DIFF_QK_DIM)), g)
    return t if pos is None else rope_2d(t, *pos)


def diff_attend(q, k, v, lam, lam_init, g_sub):
    a1 = attend(q[..., 0, :], k[..., 0, :], v)
    a2 = attend(q[..., 1, :], k[..., 1, :], v)
    o = a1 - lam.astype(a1.dtype) * a2
    return merge_heads(rms_norm(o, g_sub) * (1 - lam_init))


def diff_mixer(p, pc, g_q, g_k, lam_p, g_sub, lam_init, pos, need_ctx):
    lp = lam_p.astype(jnp.float32)
    lam = jnp.exp(jnp.sum(lp[0] * lp[1])) - jnp.exp(jnp.sum(lp[2] * lp[3])) + lam_init
    q, k = diff_qk(p[0], g_q, pos), diff_qk(p[1], g_k, pos)
    v = split_heads(p[2], N_GROUP_HEADS)
    kc, vc = diff_qk(pc[1], g_k, None), split_heads(pc[2], N_GROUP_HEADS)
    o = diff_attend(q, jnp.concatenate([kc, k], axis=1), jnp.concatenate([vc, v], axis=1), lam, lam_init, g_sub)
    oc = None
    if need_ctx:
        oc = diff_attend(diff_qk(pc[0], g_q, None), kc, vc, lam, lam_init, g_sub)
    return o, oc


def gqa_kv(pk, pv, g_k, pos):
    rep = N_GROUP_HEADS // GQA_KV_HEADS
    k = rms_norm(split_heads(pk, GQA_KV_HEADS), g_k)
    if pos is not None:
        k = rope_2d(k, *pos)
    return jnp.repeat(k, rep, axis=2), jnp.repeat(split_heads(pv, GQA_KV_HEADS), rep, axis=2)


def gqa_q(pq, g_q, pos):
    q = rms_norm(split_heads(pq, N_GROUP_HEADS), g_q)
    return q if pos is None else rope_2d(q, *pos)


def gqa_mixer(p, pc, g_q, g_k, pos, need_ctx):
    q = gqa_q(p[0], g_q, pos)
    k, v = gqa_kv(p[1], p[2], g_k, pos)
    kc, vc = gqa_kv(pc[1], pc[2], g_k, None)
    o = merge_heads(attend(q, jnp.concatenate([kc, k], axis=1), jnp.concatenate([vc, v], axis=1)))
    oc = merge_heads(attend(gqa_q(pc[0], g_q, None), kc, vc)) if need_ctx else None
    return o, oc


def swiglu(xe, wg, wu, wd):
    return (jax.nn.silu(xe @ wg) * (xe @ wu)) @ wd


def expert_choice_ffn(h, w_router, w_gate, w_up, w_down):
    B, n, D = h.shape
    cap = EC_CAPACITY * n // N_EXPERTS
    logits = jnp.einsum('bnd,de->ben', h, w_router, preferred_element_type=jnp.float32)
    aff = jax.nn.softmax(logits, axis=1)
    gate, idx = lax.top_k(aff, cap)
    xe = jax.vmap(lambda hb, ib: hb[ib])(h, idx)
    ye = lax.map(lambda a: swiglu(*a), (jnp.swapaxes(xe, 0, 1), w_gate, w_up, w_down))
    ye = jnp.swapaxes(ye, 0, 1) * gate[..., None].astype(h.dtype)

    def combine(ib, yb):
        return jnp.zeros((n, D), yb.dtype).at[ib.reshape(-1)].add(yb.reshape(-1, D))

    return jax.vmap(combine)(idx, ye)


def setup_inputs(seed: int = 0) -> dict:
    key = jax.random.key(seed)
    ks = iter(jax.random.split(key, 32))
    L, D, H = DEPTH, D_MODEL, N_GROUP_HEADS

    def nrm(shape, s):
        return jax.random.normal(next(ks), shape, jnp.float32) * s

    def gain(shape):
        return 1.0 + nrm(shape, 0.02)

    return {
        'x': nrm((BATCH, SEQ, D), 1.0),
        'c': nrm((BATCH, D), 1.0),
        'ctx': nrm((BATCH, CTX_LEN, D), 1.0),
        'c_ctx': nrm((D,), 1.0),
        'w_mod': nrm((L, D, N_MOD * D), 0.5 * D ** -0.5),
        'b_mod': nrm((L, N_MOD * D), 0.01),
        'g_attn': gain((L, D)),
        'g_ffn': gain((L, D)),
        'w_in': nrm((L, D, D_IN), D ** -0.5),
        'g_na_q': gain((L, HEAD_DIM)),
        'g_na_k': gain((L, HEAD_DIM)),
        'na_rpb': nrm((L, H, 2 * NA_WIN_H - 1, 2 * NA_WIN_W - 1), 0.1),
        'g_mla_cq': gain((L, MLA_Q_RANK)),
        'w_mla_uq': nrm((L, MLA_Q_RANK, H * (MLA_NOPE_DIM + MLA_ROPE_DIM)), MLA_Q_RANK ** -0.5),
        'g_mla_q': gain((L, MLA_NOPE_DIM + MLA_ROPE_DIM)),
        'g_mla_ckv': gain((L, MLA_KV_RANK)),
        'w_mla_ukv': nrm((L, MLA_KV_RANK, H * (MLA_NOPE_DIM + MLA_V_DIM)), MLA_KV_RANK ** -0.5),
        'g_mla_k_nope': gain((L, MLA_NOPE_DIM)),
        'g_mla_k_rope': gain((L, MLA_ROPE_DIM)),
        'g_diff_q': gain((L, 2, DIFF_QK_DIM)),
        'g_diff_k': gain((L, 2, DIFF_QK_DIM)),
        'diff_lambda': nrm((L, 4, DIFF_QK_DIM), 0.1),
        'g_diff_sub': gain((L, DIFF_V_DIM)),
        'g_gqa_q': gain((L, HEAD_DIM)),
        'g_gqa_k': gain((L, HEAD_DIM)),
        'w_out': nrm((L, D, D), D ** -0.5),
        'w_router': nrm((L, D, N_EXPERTS), D ** -0.5),
        'w_gate': nrm((L, N_EXPERTS, D, D_FF_EXPERT), D ** -0.5),
        'w_up': nrm((L, N_EXPERTS, D, D_FF_EXPERT), D ** -0.5),
        'w_down': nrm((L, N_EXPERTS, D_FF_EXPERT, D), D_FF_EXPERT ** -0.5),
    }


def reference(x, c, ctx, c_ctx, w_mod, b_mod, g_attn, g_ffn, w_in, g_na_q, g_na_k, na_rpb,
              g_mla_cq, w_mla_uq, g_mla_q, g_mla_ckv, w_mla_ukv, g_mla_k_nope, g_mla_k_rope,
              g_diff_q, g_diff_k, diff_lambda, g_diff_sub, g_gqa_q, g_gqa_k, w_out,
              w_router, w_gate, w_up, w_down):
    B, S, D = x.shape
    rows = S // GRID_W
    t = jnp.arange(S, dtype=jnp.int32)
    pos = (t // GRID_W, t % GRID_W)
    xc = ctx
    silu_c = jax.nn.silu(c)
    silu_cc = jax.nn.silu(c_ctx)[None]
    for l in range(DEPTH):
        need_ctx = l < DEPTH - 1
        lam_init = 0.8 - 0.6 * math.exp(-0.3 * l)
        mod = jnp.split((silu_c @ w_mod[l] + b_mod[l])[:, None, :], N_MOD, axis=-1)
        mod_c = jnp.split((silu_cc @ w_mod[l] + b_mod[l])[:, None, :], N_MOD, axis=-1)

        p = split_cols(modulate(x, g_attn[l], mod[0], mod[1]) @ w_in[l])
        pc = split_cols(modulate(xc, g_attn[l], mod_c[0], mod_c[1]) @ w_in[l])
        o_na, oc_na = na_mixer(p[0:3], pc[0:3], g_na_q[l], g_na_k[l], na_rpb[l], rows, need_ctx)
        o_mla, oc_mla = mla_mixer(p[3:6], pc[3:6], g_mla_cq[l], w_mla_uq[l], g_mla_q[l], g_mla_ckv[l],
                                  w_mla_ukv[l], g_mla_k_nope[l], g_mla_k_rope[l], pos, need_ctx)
        o_diff, oc_diff = diff_mixer(p[6:9], pc[6:9], g_diff_q[l], g_diff_k[l], diff_lambda[l],
                                     g_diff_sub[l], lam_init, pos, need_ctx)
        o_gqa, oc_gqa = gqa_mixer(p[9:12], pc[9:12], g_gqa_q[l], g_gqa_k[l], pos, need_ctx)
        x = x + mod[2] * (jnp.concatenate([o_na, o_mla, o_diff, o_gqa], axis=-1) @ w_out[l])
        if need_ctx:
            xc = xc + mod_c[2] * (jnp.concatenate([oc_na, oc_mla, oc_diff, oc_gqa], axis=-1) @ w_out[l])

        x = x + mod[5] * expert_choice_ffn(modulate(x, g_ffn[l], mod[3], mod[4]),
                                           w_router[l], w_gate[l], w_up[l], w_down[l])
        if need_ctx:
            xc = xc + mod_c[5] * expert_choice_ffn(modulate(xc, g_ffn[l], mod_c[3], mod_c[4]),
                                                   w_router[l], w_gate[l], w_up[l], w_down[l])
    return x
```

```python
import contextlib
import math
import numpy as np
import ml_dtypes
import concourse.bass as bass
import concourse.mybir as mybir
from concourse.bass_utils import run_bass_kernel_spmd

F32 = mybir.dt.float32
BF16 = mybir.dt.bfloat16
AF = mybir.ActivationFunctionType
ALU = mybir.AluOpType
AX = mybir.AxisListType
NPBF = ml_dtypes.bfloat16

D = 1024
SEQ = 16384
NCORE = 8
NT = 4096
NCTX = 256
NTOK = NT + NCTX
NTL = NTOK // 128
NKEY = SEQ + NCTX
NKT = NKEY // 128
DIN = 2464
DFF = 2816
NE = 16
EPS = 1e-6
QF_W = 1152
KF_W = 1024
NVH = 14
VF_W = NVH * 65
NA_HALO = 3
NA_T = 2 + 32 + 2 * NA_HALO


SEM_ROTATE = 30000


class KB:
    def __init__(self, nc, stack):
        self.nc = nc
        self.stack = stack
        self.eng = {"pe": nc.tensor, "act": nc.scalar, "dve": nc.vector,
                    "pool": nc.gpsimd, "sp": nc.sync}
        self.nsem = 0
        self.cur = {}
        self.pe_sems = set()
        for e in self.eng:
            self.cur[e] = [self._newsem(e), 0]
            if e == "pe":
                self.pe_sems.add(id(self.cur[e][0]))
        self.waited = {}
        self.last_w = {}
        self.readers = {}
        self.dma_sems = {}
        self.n_instr = 0

    def _newsem(self, tag):
        self.nsem += 1
        return self.stack.enter_context(self.nc.semaphore(f"s{self.nsem}_{tag}"))

    def _wait(self, e, sem, val):
        if e == "pe" and id(sem) in self.pe_sems:
            return
        key = (e, id(sem))
        if self.waited.get(key, 0) >= val:
            return
        self.waited[key] = val
        self.eng[e].wait_ge(sem, val)
        self.n_instr += 1

    def _deps(self, e, reads, writes):
        need = {}

        def add(rec):
            if rec is None:
                return
            sem, val = rec
            k = id(sem)
            if k not in need or need[k][1] < val:
                need[k] = (sem, val)
        for r in reads:
            add(self.last_w.get(r))
        for w in writes:
            add(self.last_w.get(w))
            for rec in self.readers.get(w, ()):
                add(rec)
        for sem, val in need.values():
            self._wait(e, sem, val)

    def _commit(self, rec, reads, writes):
        for r in reads:
            lst = self.readers.setdefault(r, [])
            lst.append(rec)
            if len(lst) > 8:
                best = {}
                for s, v in lst:
                    if id(s) not in best or best[id(s)][1] < v:
                        best[id(s)] = (s, v)
                self.readers[r] = list(best.values())
        for w in writes:
            self.last_w[w] = rec
            self.readers[w] = []

    def op(self, e, fn, reads=(), writes=()):
        self._deps(e, reads, writes)
        ins = fn(self.eng[e])
        st = self.cur[e]
        if st[1] >= SEM_ROTATE:
            st[0] = self._newsem(e)
            st[1] = 0
            if e == "pe":
                self.pe_sems.add(id(st[0]))
        st[1] += 1
        ins.then_inc(st[0], 1)
        self.n_instr += 1
        self._commit((st[0], st[1]), reads, writes)
        return ins

    def dma(self, q, out, in_, reads=(), writes=(), slot=None, **kw):
        self._deps(q, reads, writes)
        if slot is None:
            slot = ("auto",) + tuple(writes) + tuple(reads)
        st = self.dma_sems.get(slot)
        if st is None or st[1] >= SEM_ROTATE:
            st = [self._newsem("d"), 0]
            self.dma_sems[slot] = st
        ins = self.eng[q].dma_start(out=out, in_=in_, **kw)
        st[1] += 16
        ins.then_inc(st[0], 16)
        self.n_instr += 1
        self._commit((st[0], st[1]), reads, writes)
        return ins

    def wait_all(self, e):
        recs = {}
        for rec in list(self.last_w.values()) + [x for l in self.readers.values() for x in l]:
            s, v = rec
            if id(s) not in recs or recs[id(s)][1] < v:
                recs[id(s)] = (s, v)
        for s, v in recs.values():
            self._wait(e, s, v)


class Ctx:
    def __init__(self):
        self.nc = bass.Bass("TRN2", target_bir_lowering=False)
        self.stack = contextlib.ExitStack()
        self.kb = KB(self.nc, self.stack)
        self._n = 0

    def din(self, name, shape, dt=F32):
        return self.nc.dram_tensor(name, list(shape), dt, kind="ExternalInput").ap()

    def dout(self, name, shape, dt=F32):
        return self.nc.dram_tensor(name, list(shape), dt, kind="ExternalOutput").ap()

    def dint(self, name, shape, dt=F32):
        return self.nc.dram_tensor(name, list(shape), dt, kind="Internal").ap()

    def sb(self, name, shape, dt=F32, stack=None):
        return (stack or self.stack).enter_context(self.nc.sbuf_tensor(name, list(shape), dt))

    def ps(self, name, shape, dt=F32, stack=None):
        return (stack or self.stack).enter_context(self.nc.psum_tensor(name, list(shape), dt))

    def barrier(self):
        for e in self.kb.eng:
            self.kb.wait_all(e)

    def finish(self):
        self.kb.wait_all("sp")
        self.stack.close()
        return self.nc


def make_ident(cx, name="ident"):
    kb = cx.kb
    identf = cx.sb(name + "_f", [128, 128], F32)
    identb = cx.sb(name + "_b", [128, 128], BF16)
    kb.op("pool", lambda e: e.memset(identf[:], 1.0), writes=[name + "_f"])
    kb.op("pool", lambda e: e.affine_select(out=identf[:], in_=identf[:], pattern=[[-1, 128]],
                                            compare_op=ALU.is_equal, fill=0.0, base=0, channel_multiplier=1),
          reads=[name + "_f"], writes=[name + "_f"])
    kb.op("dve", lambda e: e.tensor_copy(out=identb[:], in_=identf[:]), reads=[name + "_f"], writes=[name + "_b"])
    return identf, identb


def rstd_from_ss(cx, ss_ap, out_ap, width, eps_ap, rkeys, wkey):
    kb = cx.kb
    kb.op("act", lambda e: e.activation(out=out_ap, in_=ss_ap, func=AF.Sqrt, scale=1.0 / width, bias=eps_ap),
          reads=list(rkeys) + ["eps"], writes=[wkey])
    kb.op("dve", lambda e: e.reciprocal(out=out_ap, in_=out_ap), reads=[wkey], writes=[wkey])


def build_P():
    cx = Ctx()
    nc, kb = cx.nc, cx.kb
    xin = cx.din("xin", [NTOK, D])
    cs2 = cx.din("cs2", [2, D])
    wmod = cx.din("wmod", [D, 6 * D])
    bmod = cx.din("bmod", [1, 6 * D])
    gattn = cx.din("gattn", [1, D])
    win = cx.din("win", [D, DIN])
    gains = cx.din("gains", [1, DIN])
    wuq = cx.din("wuq", [256, 384])
    wukv = cx.din("wukv", [128, 512])
    rope = cx.din("rope", [NT, 192])
    modv = cx.dout("modv", [2, 6 * D])
    qT_o = cx.dout("qT", [QF_W, NTOK], BF16)
    kT_o = cx.dout("kT", [KF_W, NTOK], BF16)
    v_o = cx.dout("v", [NTL, 128, VF_W], BF16)

    identf, identb = make_ident(cx)
    eps = cx.sb("eps", [128, 1])
    kb.op("dve", lambda e: e.memset(eps[:], EPS), writes=["eps"])

    Abc = cx.sb("Abc", [128, 2, D])
    Bbc = cx.sb("Bbc", [128, 2, D])
    with contextlib.ExitStack() as ph:
        cs_sb = cx.sb("cs_sb", [2, D], stack=ph)
        bm_sb = cx.sb("bm_sb", [2, 6 * D], stack=ph)
        modrow = cx.sb("modrow", [2, 6 * D], stack=ph)
        scT = cx.sb("scT", [128, 8, 2], stack=ph)
        wm = [cx.sb(f"wm{i}", [128, 8, 512], stack=ph) for i in range(2)]
        gb = cx.sb("gb", [128, D], stack=ph)
        tmpb = cx.sb("tmpb", [128, D], stack=ph)
        ps_t = cx.ps("ps_mt", [128, 512], stack=ph)
        ps_m = [cx.ps(f"ps_mm{i}", [128, 512], stack=ph) for i in range(2)]
        kb.dma("sp", cs_sb[:], cs2[:, :], writes=["cs_sb"])
        kb.dma("sp", bm_sb[:], bmod[0:1, :].broadcast_to([2, 6 * D]), writes=["bm_sb"])
        kb.op("act", lambda e: e.activation(out=cs_sb[:], in_=cs_sb[:], func=AF.Silu), reads=["cs_sb"], writes=["cs_sb"])
        for c in range(8):
            kb.op("pe", lambda e: e.transpose(out=ps_t[:, c * 2:c * 2 + 2], in_=cs_sb[0:2, c * 128:(c + 1) * 128],
                                              identity=identf[0:2, 0:2]),
                  reads=["cs_sb", "ident_f"], writes=[("ps_mt", c)])
        kb.op("dve", lambda e: e.tensor_copy(out=scT[:].rearrange("p c r -> p (c r)"), in_=ps_t[:, 0:16]),
              reads=[("ps_mt", c) for c in range(8)], writes=["scT"])
        wm_v = wmod.rearrange("(c p) n -> p c n", p=128)
        for cb in range(12):
            s = cb % 2
            kb.dma("sp" if s == 0 else "pool", wm[s][:], wm_v[:, :, cb * 512:(cb + 1) * 512], writes=[("wm", s)], slot=("wm", s))
            for c in range(8):
                kb.op("pe", lambda e: e.matmul(ps_m[s][0:2, :], lhsT=scT[:, c, :], rhs=wm[s][:, c, :],
                                               start=(c == 0), stop=(c == 7)),
                      reads=[("wm", s), "scT"], writes=[("ps_mm", s)])
            kb.op("dve", lambda e: e.tensor_tensor(out=modrow[:, cb * 512:(cb + 1) * 512], in0=ps_m[s][0:2, :],
                                                   in1=bm_sb[:, cb * 512:(cb + 1) * 512], op=ALU.add),
                  reads=[("ps_mm", s), "bm_sb"], writes=["modrow"])
        kb.dma("sp", modv[:, :], modrow[:], reads=["modrow"], writes=["modv_d"])
        kb.dma("sp", gb[:], gattn[0:1, :].broadcast_to([128, D]), writes=["gb"])
        for m in range(2):
            kb.dma("sp", tmpb[:], modv[m:m + 1, D:2 * D].broadcast_to([128, D]), reads=["modv_d"], writes=["tmpb"])
            kb.op("dve", lambda e: e.scalar_tensor_tensor(out=Abc[:, m, :], in0=tmpb[:], scalar=1.0, in1=gb[:],
                                                          op0=ALU.add, op1=ALU.mult),
                  reads=["tmpb", "gb"], writes=[("Abc", m)])
            kb.dma("pool", Bbc[:, m, :], modv[m:m + 1, 0:D].broadcast_to([128, D]), reads=["modv_d"], writes=[("Bbc", m)])
        cx.barrier()

    win_bf = cx.sb("win_bf", [128, 8, DIN], BF16)
    wuq_bf = cx.sb("wuq_bf", [128, 2, 384], BF16)
    wukv_bf = cx.sb("wukv_bf", [128, 512], BF16)
    gains_sb = cx.sb("gains_sb", [128, DIN])
    kb.dma("pool", gains_sb[:], gains[0:1, :].broadcast_to([128, DIN]), writes=["gains"])
    with contextlib.ExitStack() as ph:
        wst = [cx.sb(f"wst{i}", [128, DIN], stack=ph) for i in range(2)]
        for c in range(8):
            s = c % 2
            kb.dma("sp" if s == 0 else "pool", wst[s][:], win[c * 128:(c + 1) * 128, :], writes=[("wst", s)], slot=("wst", s))
            kb.op("dve" if s == 0 else "pool", lambda e: e.tensor_copy(out=win_bf[:, c, :], in_=wst[s][:]),
                  reads=[("wst", s)], writes=["win_bf"])
        for c in range(2):
            kb.dma("sp", wst[0][:, 0:384], wuq[c * 128:(c + 1) * 128, :], writes=[("wst", 0)], slot=("wst", 0))
            kb.op("dve", lambda e: e.tensor_copy(out=wuq_bf[:, c, :], in_=wst[0][:, 0:384]), reads=[("wst", 0)], writes=["wuq_bf"])
        kb.dma("sp", wst[1][:, 0:512], wukv[:, :], writes=[("wst", 1)], slot=("wst", 1))
        kb.op("dve", lambda e: e.tensor_copy(out=wukv_bf[:], in_=wst[1][:, 0:512]), reads=[("wst", 1)], writes=["wukv_bf"])
        cx.barrier()

    xt = [cx.sb(f"xt{i}", [128, D]) for i in range(2)]
    rp = [cx.sb(f"rp{i}", [128, 192]) for i in range(2)]
    junk = cx.sb("junk", [128, D])
    st_ss = cx.sb("st_ss", [128, 1])
    st_rstd = cx.sb("st_rstd", [128, 1])
    tmp = cx.sb("tmp", [128, D])
    hb = cx.sb("hb", [128, D], BF16)
    hT = cx.sb("hT", [128, D], BF16)
    pj = cx.sb("pj", [128, DIN])
    sq = cx.sb("sq", [128, 544])
    nss = cx.sb("nss", [128, 32])
    nrs = cx.sb("nrs", [128, 32])
    ntmp = cx.sb("ntmp", [128, 544])
    n32 = cx.sb("n32", [128, 544])
    n64 = cx.sb("n64", [128, 384])
    cqn = cx.sb("cqn", [128, 256], BF16)
    ckvn = cx.sb("ckvn", [128, 128], BF16)
    cT = cx.sb("cT", [128, 384], BF16)
    mq = cx.sb("mq", [128, 384])
    mqn = cx.sb("mqn", [128, 384])
    mkv = cx.sb("mkv", [128, 512])
    r1 = cx.sb("r1", [128, 544])
    r2 = cx.sb("r2", [128, 544])
    QF = [cx.sb(f"QF{i}", [128, QF_W], BF16) for i in range(2)]
    KF = [cx.sb(f"KF{i}", [128, KF_W], BF16) for i in range(2)]
    VF = [cx.sb(f"VF{i}", [128, NVH, 65], BF16) for i in range(2)]
    sQ = [cx.sb(f"sQ{i}", [128, 9, 512], BF16) for i in range(2)]
    sK = [cx.sb(f"sK{i}", [128, 8, 512], BF16) for i in range(2)]
    ps_tr = cx.ps("ps_tr", [128, D], BF16)
    ps_pj = [cx.ps(f"ps_pj{i}", [128, 512]) for i in range(2)]
    ps_c = cx.ps("ps_c", [128, 1024], BF16)
    ps_up = cx.ps("ps_up", [128, 512])
    ps_q = [cx.ps(f"ps_q{i}", [128, 1024], BF16) for i in range(3)]

    for i in range(2):
        kb.op("pool", lambda e: e.memset(KF[i][:], 0.0), writes=[("KF", i)])
        kb.op("pool", lambda e: e.memset(VF[i][:], 1.0), writes=[("VF", i)])

    GO = {"naqk": 0, "w32": 512, "gqa": 1056, "nope": 1440, "cq": 1696, "ckv": 1952, "mq": 2080}

    def gnorm(src3, G, w, rkeys, scratch_w):
        sq3 = sq[:, 0:G * w].rearrange("p (g w) -> p g w", w=w)
        kb.op("dve", lambda e: e.tensor_tensor(out=sq3, in0=src3, in1=src3, op=ALU.mult), reads=rkeys, writes=["sq"])
        kb.op("dve", lambda e: e.tensor_reduce(out=nss[:, 0:G], in_=sq3, axis=AX.X, op=ALU.add), reads=["sq"], writes=["nss"])
        rstd_from_ss(cx, nss[:, 0:G], nrs[:, 0:G], w, eps[:, 0:1], ["nss"], "nrs")
        o3 = ntmp[:, 0:G * w].rearrange("p (g w) -> p g w", w=w)
        kb.op("dve", lambda e: e.tensor_tensor(out=o3, in0=src3, in1=nrs[:, 0:G].unsqueeze(2).broadcast_to([128, G, w]),
                                               op=ALU.mult), reads=list(rkeys) + ["nrs"], writes=["ntmp"])
        return o3

    def gain3(off, G, w):
        return gains_sb[:, off:off + G * w].rearrange("p (g w) -> p g w", w=w)

    def rope_apply(src3, G, n, tab, toff, dsts, rkeys, rpk):
        C = tab[:, toff:toff + n].unsqueeze(1).broadcast_to([128, G, n])
        q4 = n // 4
        t1 = r1[:, 0:G * n].rearrange("p (g w) -> p g w", w=n)
        t2 = r2[:, 0:G * n].rearrange("p (g w) -> p g w", w=n)
        kb.op("dve", lambda e: e.tensor_tensor(out=t1, in0=src3, in1=C, op=ALU.mult), reads=list(rkeys) + [rpk], writes=["r1"])
        s5 = src3.rearrange("p g (r x w) -> p g r x w", r=2, x=2, w=q4)
        t5 = t2.rearrange("p g (r x w) -> p g r x w", r=2, x=2, w=q4)
        S5 = tab[:, toff + n:toff + 2 * n].rearrange("p (r x w) -> p r x w", r=2, x=2, w=q4)
        for xh in range(2):
            Sx = S5[:, :, xh, :].unsqueeze(1).broadcast_to([128, G, 2, q4])
            kb.op("dve", lambda e: e.tensor_tensor(out=t5[:, :, :, xh, :], in0=s5[:, :, :, 1 - xh, :], in1=Sx, op=ALU.mult),
                  reads=list(rkeys) + [rpk], writes=[("r2", xh)])
        for (g0, g1, dst3, wkey) in dsts:
            kb.op("dve", lambda e: e.tensor_tensor(out=dst3, in0=t1[:, g0:g1, :], in1=t2[:, g0:g1, :], op=ALU.add),
                  reads=["r1", ("r2", 0), ("r2", 1)], writes=[wkey])

    rope_v = rope
    qT_v = qT_o.rearrange("(k p) t -> p k t", p=128)
    kT_v = kT_o.rearrange("(k p) t -> p k t", p=128)

    for t in range(NTL):
        m = 0 if t < NT // 128 else 1
        s = t % 2
        grp = t // 4
        gs = grp % 2
        tin = t % 4
        dq = "sp" if s == 0 else "pool"
        kb.dma(dq, xt[s][:], xin[t * 128:(t + 1) * 128, :], writes=[("xt", s)], slot=("xt", s))
        if m == 0:
            kb.dma(dq, rp[s][:], rope_v[t * 128:(t + 1) * 128, :], writes=[("rp", s)], slot=("rp", s))
        kb.op("act", lambda e: e.activation(out=junk[:], in_=xt[s][:], func=AF.Square, accum_out=st_ss[:]),
              reads=[("xt", s)], writes=["junk", "st_ss"])
        rstd_from_ss(cx, st_ss[:], st_rstd[:], D, eps[:, 0:1], ["st_ss"], "st_rstd")
        kb.op("dve", lambda e: e.scalar_tensor_tensor(out=tmp[:], in0=xt[s][:], scalar=st_rstd[:, 0:1], in1=Abc[:, m, :],
                                                      op0=ALU.mult, op1=ALU.mult),
              reads=[("xt", s), "st_rstd", ("Abc", m)], writes=["tmp"])
        kb.op("pool", lambda e: e.tensor_tensor(out=hb[:], in0=tmp[:], in1=Bbc[:, m, :], op=ALU.add),
              reads=["tmp", ("Bbc", m)], writes=["hb"])
        for c in range(8):
            kb.op("pe", lambda e: e.transpose(out=ps_tr[:, c * 128:(c + 1) * 128], in_=hb[:, c * 128:(c + 1) * 128], identity=identb[:]),
                  reads=["hb", "ident_b"], writes=[("ps_tr", c)])
        kb.op("act", lambda e: e.copy(out=hT[:], in_=ps_tr[:]), reads=[("ps_tr", c) for c in range(8)], writes=["hT"])
        for cb in range(5):
            c0 = cb * 512
            wd = min(512, DIN - c0)
            pp = ps_pj[cb % 2]
            for c in range(8):
                kb.op("pe", lambda e: e.matmul(pp[:, 0:wd], lhsT=hT[:, c * 128:(c + 1) * 128], rhs=win_bf[:, c, c0:c0 + wd],
                                               start=(c == 0), stop=(c == 7)),
                      reads=["hT", "win_bf"], writes=[("ps_pj", cb % 2)])
            kb.op("act", lambda e: e.copy(out=pj[:, c0:c0 + wd], in_=pp[:, 0:wd]), reads=[("ps_pj", cb % 2)], writes=[("pj", cb)])
        PJ = [("pj", cb) for cb in range(5)]
        qf, kf, vf = QF[s], KF[s], VF[s]
        qk, kk, vk = ("QF", s), ("KF", s), ("VF", s)
        o3 = gnorm(pj[:, 0:512].rearrange("p (g w) -> p g w", w=64), 8, 64, PJ, 512)
        kb.op("dve", lambda e: e.tensor_tensor(out=qf[:, 0:256].rearrange("p (g w) -> p g w", w=64), in0=o3[:, 0:4, :],
                                               in1=gain3(GO["naqk"], 8, 64)[:, 0:4, :], op=ALU.mult),
              reads=["ntmp", "gains"], writes=[qk])
        kb.op("dve", lambda e: e.tensor_tensor(out=kf[:, 0:256].rearrange("p (g w) -> p g w", w=64), in0=o3[:, 4:8, :],
                                               in1=gain3(GO["naqk"], 8, 64)[:, 4:8, :], op=ALU.mult),
              reads=["ntmp", "gains"], writes=[kk])
        o3 = gnorm(pj[:, 1152:1696].rearrange("p (g w) -> p g w", w=32), 17, 32, PJ, 544)
        n32_3 = n32[:, 0:544].rearrange("p (g w) -> p g w", w=32)
        d_kr = kf[:, 512:544].rearrange("p (g w) -> p g w", w=32)
        d_dq = qf[:, 640:896].rearrange("p (g w) -> p g w", w=32)
        d_dk = kf[:, 640:896].rearrange("p (g w) -> p g w", w=32)
        if m == 0:
            kb.op("dve", lambda e: e.tensor_tensor(out=n32_3, in0=o3, in1=gain3(GO["w32"], 17, 32), op=ALU.mult),
                  reads=["ntmp", "gains"], writes=["n32"])
            rope_apply(n32_3, 17, 32, rp[s], 0, [(0, 1, d_kr, kk), (1, 9, d_dq, qk), (9, 17, d_dk, kk)], ["n32"], ("rp", s))
        else:
            g3 = gain3(GO["w32"], 17, 32)
            for (g0, g1, dst, wk) in [(0, 1, d_kr, kk), (1, 9, d_dq, qk), (9, 17, d_dk, kk)]:
                kb.op("dve", lambda e: e.tensor_tensor(out=dst, in0=o3[:, g0:g1, :], in1=g3[:, g0:g1, :], op=ALU.mult),
                      reads=["ntmp", "gains"], writes=[wk])
        o3 = gnorm(pj[:, 1952:2336].rearrange("p (g w) -> p g w", w=64), 6, 64, PJ, 384)
        n64_3 = n64[:, 0:384].rearrange("p (g w) -> p g w", w=64)
        d_gq = qf[:, 896:1152].rearrange("p (g w) -> p g w", w=64)
        d_gk = kf[:, 896:1024].rearrange("p (g w) -> p g w", w=64)
        if m == 0:
            kb.op("dve", lambda e: e.tensor_tensor(out=n64_3, in0=o3, in1=gain3(GO["gqa"], 6, 64), op=ALU.mult),
                  reads=["ntmp", "gains"], writes=["n64"])
            rope_apply(n64_3, 6, 64, rp[s], 64, [(0, 4, d_gq, qk), (4, 6, d_gk, kk)], ["n64"], ("rp", s))
        else:
            g3 = gain3(GO["gqa"], 6, 64)
            for (g0, g1, dst, wk) in [(0, 4, d_gq, qk), (4, 6, d_gk, kk)]:
                kb.op("dve", lambda e: e.tensor_tensor(out=dst, in0=o3[:, g0:g1, :], in1=g3[:, g0:g1, :], op=ALU.mult),
                      reads=["ntmp", "gains"], writes=[wk])
        o3 = gnorm(pj[:, 768:1024].rearrange("p (g w) -> p g w", w=256), 1, 256, PJ, 256)
        kb.op("dve", lambda e: e.tensor_tensor(out=cqn[:].rearrange("p (g w) -> p g w", w=256), in0=o3, in1=gain3(GO["cq"], 1, 256), op=ALU.mult),
              reads=["ntmp", "gains"], writes=["cqn"])
        for c in range(2):
            kb.op("pe", lambda e: e.transpose(out=ps_c[:, c * 128:(c + 1) * 128], in_=cqn[:, c * 128:(c + 1) * 128], identity=identb[:]),
                  reads=["cqn", "ident_b"], writes=[("ps_c", c)])
        kb.op("act", lambda e: e.copy(out=cT[:, 0:256], in_=ps_c[:, 0:256]), reads=[("ps_c", 0), ("ps_c", 1)], writes=[("cT", 0)])
        for c in range(2):
            kb.op("pe", lambda e: e.matmul(ps_up[:, 0:384], lhsT=cT[:, c * 128:(c + 1) * 128], rhs=wuq_bf[:, c, :], start=(c == 0), stop=(c == 1)),
                  reads=[("cT", 0), "wuq_bf"], writes=["ps_up"])
        kb.op("act", lambda e: e.copy(out=mq[:], in_=ps_up[:, 0:384]), reads=["ps_up"], writes=["mq"])
        o3 = gnorm(mq[:, 0:384].rearrange("p (g w) -> p g w", w=96), 4, 96, ["mq"], 384)
        mqn3 = mqn[:, 0:384].rearrange("p (g w) -> p g w", w=96)
        d_mq = qf[:, 256:640].rearrange("p (g w) -> p g w", w=96)
        if m == 0:
            kb.op("dve", lambda e: e.tensor_tensor(out=mqn3, in0=o3, in1=gain3(GO["mq"], 4, 96), op=ALU.mult),
                  reads=["ntmp", "gains"], writes=["mqn"])
            kb.op("pool", lambda e: e.tensor_copy(out=d_mq[:, :, 0:64], in_=mqn3[:, :, 0:64]), reads=["mqn"], writes=[qk])
            rope_apply(mqn3[:, :, 64:96], 4, 32, rp[s], 0, [(0, 4, d_mq[:, :, 64:96], qk)], ["mqn"], ("rp", s))
        else:
            kb.op("dve", lambda e: e.tensor_tensor(out=d_mq, in0=o3, in1=gain3(GO["mq"], 4, 96), op=ALU.mult),
                  reads=["ntmp", "gains"], writes=[qk])
        o3 = gnorm(pj[:, 1024:1152].rearrange("p (g w) -> p g w", w=128), 1, 128, PJ, 128)
        kb.op("dve", lambda e: e.tensor_tensor(out=ckvn[:].rearrange("p (g w) -> p g w", w=128), in0=o3, in1=gain3(GO["ckv"], 1, 128), op=ALU.mult),
              reads=["ntmp", "gains"], writes=["ckvn"])
        kb.op("pe", lambda e: e.transpose(out=ps_c[:, 256:384], in_=ckvn[:], identity=identb[:]),
              reads=["ckvn", "ident_b"], writes=[("ps_c", 2)])
        kb.op("act", lambda e: e.copy(out=cT[:, 256:384], in_=ps_c[:, 256:384]), reads=[("ps_c", 2)], writes=[("cT", 1)])
        kb.op("pe", lambda e: e.matmul(ps_up[:, 0:512], lhsT=cT[:, 256:384], rhs=wukv_bf[:], start=True, stop=True),
              reads=[("cT", 1), "wukv_bf"], writes=["ps_up"])
        kb.op("act", lambda e: e.copy(out=mkv[:], in_=ps_up[:, 0:512]), reads=["ps_up"], writes=["mkv"])
        mkv3 = mkv[:, 0:512].rearrange("p (g w) -> p g w", w=128)
        o3 = gnorm(mkv3[:, :, 0:64], 4, 64, ["mkv"], 256)
        kb.op("dve", lambda e: e.tensor_tensor(out=kf[:, 256:512].rearrange("p (g w) -> p g w", w=64), in0=o3,
                                               in1=gain3(GO["nope"], 4, 64), op=ALU.mult),
              reads=["ntmp", "gains"], writes=[kk])
        kb.op("pool", lambda e: e.tensor_copy(out=vf[:, 0:4, 0:64], in_=pj[:, 512:768].rearrange("p (g w) -> p g w", w=64)), reads=PJ, writes=[vk])
        kb.op("pool", lambda e: e.tensor_copy(out=vf[:, 4:8, 0:64], in_=mkv3[:, :, 64:128]), reads=["mkv"], writes=[vk])
        kb.op("pool", lambda e: e.tensor_copy(out=vf[:, 8:12, 0:64], in_=pj[:, 1696:1952].rearrange("p (g w) -> p g w", w=64)), reads=PJ, writes=[vk])
        kb.op("pool", lambda e: e.tensor_copy(out=vf[:, 12:14, 0:64], in_=pj[:, 2336:2464].rearrange("p (g w) -> p g w", w=64)), reads=PJ, writes=[vk])
        kb.dma(dq, v_o[t, :, :], vf[:].rearrange("p g w -> p (g w)"), reads=[vk], writes=["v_d"], slot=("vout", s))
        blocks = [(qf, qk, b, sQ[gs], ("sQ", gs)) for b in range(9)] + [(kf, kk, b, sK[gs], ("sK", gs)) for b in range(8)]
        for bi, (src, skey, b, dst, dkey) in enumerate(blocks):
            pq = ps_q[bi // 8]
            kb.op("pe", lambda e: e.transpose(out=pq[:, (bi % 8) * 128:(bi % 8 + 1) * 128], in_=src[:, b * 128:(b + 1) * 128], identity=identb[:]),
                  reads=[skey, "ident_b"], writes=[("ps_q", bi // 8, bi % 8)])
        kb.op("act", lambda e: e.copy(out=sQ[gs][:, 0:8, tin * 128:(tin + 1) * 128], in_=ps_q[0][:].rearrange("p (b w) -> p b w", w=128)),
              reads=[("ps_q", 0, j) for j in range(8)], writes=[("sQ", gs)])
        kb.op("dve", lambda e: e.tensor_copy(out=sQ[gs][:, 8, tin * 128:(tin + 1) * 128], in_=ps_q[1][:, 0:128]),
              reads=[("ps_q", 1, 0)], writes=[("sQ", gs)])
        kb.op("act", lambda e: e.copy(out=sK[gs][:, 0:7, tin * 128:(tin + 1) * 128], in_=ps_q[1][:, 128:1024].rearrange("p (b w) -> p b w", w=128)),
              reads=[("ps_q", 1, j) for j in range(1, 8)], writes=[("sK", gs)])
        kb.op("dve", lambda e: e.tensor_copy(out=sK[gs][:, 7, tin * 128:(tin + 1) * 128], in_=ps_q[2][:, 0:128]),
              reads=[("ps_q", 2, 0)], writes=[("sK", gs)])
        last_in_grp = (tin == 3) or (t == NTL - 1)
        if last_in_grp:
            ntk = (tin + 1) * 128
            t0 = grp * 512
            kb.dma("sp", qT_v[:, :, t0:t0 + ntk], sQ[gs][:, :, 0:ntk], reads=[("sQ", gs)], writes=["qT_d"], slot=("sQo", gs))
            kb.dma("pool", kT_v[:, :, t0:t0 + ntk], sK[gs][:, :, 0:ntk], reads=[("sK", gs)], writes=["kT_d"], slot=("sKo", gs))
    return cx.finish()


_NC_CACHE = {}


def _get_nc(name, builder, *args):
    key = (name,) + tuple(args)
    if key not in _NC_CACHE:
        _NC_CACHE[key] = builder(*args)
    return _NC_CACHE[key]


def _rope_tables():
    t = np.arange(SEQ)
    row, col = (t // 64).astype(np.float64), (t % 64).astype(np.float64)
    out = np.zeros((SEQ, 192), np.float32)

    def fill(n, off):
        half = n // 2
        inv = 10000.0 ** (-np.arange(0, half, 2, dtype=np.float32).astype(np.float64) / half)
        inv = inv.astype(np.float32)
        for hi, pos in enumerate((row, col)):
            ang = (pos.astype(np.float32)[:, None] * inv[None, :]).astype(np.float32)
            c, s = np.cos(ang), np.sin(ang)
            q4 = n // 4
            b = off + hi * half
            out[:, b:b + q4] = c
            out[:, b + q4:b + 2 * q4] = c
            out[:, off + n + hi * half:off + n + hi * half + q4] = -s
            out[:, off + n + hi * half + q4:off + n + hi * half + 2 * q4] = s
    fill(32, 0)
    fill(64, 64)
    return out


def _gains_vec(inp, l):
    f = lambda k: np.asarray(inp[k][l], np.float32).reshape(-1)
    parts = [np.tile(f("g_na_q"), 4), np.tile(f("g_na_k"), 4),
             f("g_mla_k_rope"), np.tile(f("g_diff_q"), 4), np.tile(f("g_diff_k"), 4),
             np.tile(f("g_gqa_q"), 4), np.tile(f("g_gqa_k"), 2),
             np.tile(f("g_mla_k_nope"), 4), f("g_mla_cq"), f("g_mla_ckv"), np.tile(f("g_mla_q"), 4)]
    v = np.concatenate(parts)[None, :]
    assert v.shape == (1, DIN)
    return np.ascontiguousarray(v)


def run_P(inp, l, x_cur, xc_cur, rope_tab):
    nc = _get_nc("P", build_P)
    in_maps = []
    for c in range(NCORE):
        b, r = c // 4, c % 4
        in_maps.append({
            "xin": np.ascontiguousarray(np.concatenate([x_cur[b, r * NT:(r + 1) * NT], xc_cur[b]], axis=0)),
            "cs2": np.ascontiguousarray(np.stack([inp["c"][b], inp["c_ctx"]], axis=0)),
            "wmod": np.ascontiguousarray(inp["w_mod"][l]),
            "bmod": np.ascontiguousarray(inp["b_mod"][l][None, :]),
            "gattn": np.ascontiguousarray(inp["g_attn"][l][None, :]),
            "win": np.ascontiguousarray(inp["w_in"][l]),
            "gains": _gains_vec(inp, l),
            "wuq": np.ascontiguousarray(inp["w_mla_uq"][l]),
            "wukv": np.ascontiguousarray(inp["w_mla_ukv"][l]),
            "rope": np.ascontiguousarray(rope_tab[r * NT:(r + 1) * NT]),
        })
    res = run_bass_kernel_spmd(nc, in_maps, core_ids=list(range(NCORE)))
    return res.results


def _full_heads():
    hs = []
    for h in range(4):
        hs.append(dict(dk=96, q=[(256 + 96 * h, 96)], k=[(256 + 64 * h, 64), (512, 32)], vh=4 + h,
                       scale=96 ** -0.5, orow=256 + 64 * h, kind="plain"))
    for h in range(4):
        for c in range(2):
            hs.append(dict(dk=32, q=[(640 + 64 * h + 32 * c, 32)], k=[(640 + 64 * h + 32 * c, 32)], vh=8 + h,
                           scale=32 ** -0.5, orow=512 + 64 * h, kind="diff%d" % c))
    for h in range(4):
        hs.append(dict(dk=64, q=[(896 + 64 * h, 64)], k=[(896 + 64 * (h // 2), 64)], vh=12 + h // 2,
                       scale=64 ** -0.5, orow=768 + 64 * h, kind="plain"))
    return hs


def build_A(l, parts="fnp", nheads=16):
    need_ctx = (l == 0)
    lam_init = 0.8 - 0.6 * math.exp(-0.3 * l)
    NQ = NTOK if need_ctx else NT
    NQT = NQ // 128
    cx = Ctx()
    nc, kb = cx.nc, cx.kb
    qT = cx.din("qT", [QF_W, NTOK], BF16)
    kT = cx.din("kT", [KF_W, NKEY], BF16)
    vA = cx.din("v", [NKT, 128, VF_W], BF16)
    kna = cx.din("kna", [256, NA_T * 128], BF16)
    vna = cx.din("vna", [NA_T, 128, 4 * 65], BF16)
    nab = cx.din("nab", [4, 128, 5 * 7 * 128])
    xin = cx.din("xin", [NTOK, D])
    wout = cx.din("wout", [D, D])
    modv = cx.din("modv", [2, 6 * D])
    gffn = cx.din("gffn", [1, D])
    wr = cx.din("wr", [D, NE])
    gsub = cx.din("gsub", [64, 1])
    lamp = cx.din("lamp", [1, 128])
    x1_o = cx.dout("x1", [NQ, D])
    aff_o = cx.dout("aff", [NQ, NE])
    h2T_o = cx.dout("h2T", [D, NQ], BF16)
    oT_d = cx.dint("oT_d", [D, NQ], BF16)

    identf, identb = make_ident(cx)
    eps = cx.sb("eps", [128, 1])
    kb.op("dve", lambda e: e.memset(eps[:], EPS), writes=["eps"])
    sel65 = cx.sb("sel65", [128, 64])
    kb.op("pool", lambda e: e.memset(sel65[:], 0.0), writes=["sel65"])
    kb.op("pool", lambda e: e.memset(sel65[64:65, :], 1.0), reads=["sel65"], writes=["sel65"])
    ones64 = cx.sb("ones64", [64, 64])
    kb.op("pool", lambda e: e.memset(ones64[:], 1.0), writes=["ones64"])
    lam_sb = cx.sb("lam_sb", [64, 128])
    lam_t = cx.sb("lam_t", [64, 64])
    lam_s = cx.sb("lam_s", [64, 4])
    neglam = cx.sb("neglam", [64, 1])
    gs_sb = cx.sb("gs_sb", [64, 1])
    kb.dma("sp", lam_sb[:], lamp[0:1, :].broadcast_to([64, 128]), writes=["lam_sb"])
    kb.dma("sp", gs_sb[:], gsub[:, :], writes=["gs_sb"])
    for i in range(2):
        kb.op("dve", lambda e: e.tensor_tensor(out=lam_t[:, i * 32:(i + 1) * 32], in0=lam_sb[:, i * 64:i * 64 + 32],
                                               in1=lam_sb[:, i * 64 + 32:i * 64 + 64], op=ALU.mult),
              reads=["lam_sb"], writes=["lam_t"])
        kb.op("dve", lambda e: e.tensor_reduce(out=lam_s[:, i:i + 1], in_=lam_t[:, i * 32:(i + 1) * 32], axis=AX.X, op=ALU.add),
              reads=["lam_t"], writes=["lam_s"])
    kb.op("act", lambda e: e.activation(out=lam_s[:, 2:4], in_=lam_s[:, 0:2], func=AF.Exp), reads=["lam_s"], writes=["lam_s"])
    kb.op("dve", lambda e: e.tensor_tensor(out=neglam[:], in0=lam_s[:, 3:4], in1=lam_s[:, 2:3], op=ALU.subtract),
          reads=["lam_s"], writes=["neglam"])
    kb.op("dve", lambda e: e.tensor_scalar(out=neglam[:], in0=neglam[:], scalar1=-lam_init, scalar2=None, op0=ALU.add),
          reads=["neglam"], writes=["neglam"])
    kb.op("dve", lambda e: e.tensor_scalar(out=gs_sb[:], in0=gs_sb[:], scalar1=1.0 - lam_init, scalar2=None, op0=ALU.mult),
          reads=["gs_sb"], writes=["gs_sb"])

    with contextlib.ExitStack() as ph:
        ksb = [cx.sb(f"ksb{i}", [128, NKEY], BF16, stack=ph) for i in range(2)]
        vsb = [cx.sb(f"vsb{i}", [128, NKT, 65], BF16, stack=ph) for i in range(2)]
        qsb = [cx.sb(f"qsb{i}", [128, NTOK], BF16, stack=ph) for i in range(2)]
        pT = [cx.sb(f"pT{i}", [128, 2, 512], BF16, stack=ph) for i in range(2)]
        osb = cx.sb("osb", [65, 512], stack=ph)
        rec = cx.sb("rec", [64, 512], stack=ph)
        a1 = cx.sb("a1", [64, NTOK], stack=ph)
        dd = cx.sb("dd", [64, 512], stack=ph)
        sqd = cx.sb("sqd", [64, 512], stack=ph)
        rsd = cx.sb("rsd", [64, 512], stack=ph)
        ob = [cx.sb(f"ob{i}", [64, 512], BF16, stack=ph) for i in range(2)]
        knas = cx.sb("knas", [64, NA_T * 128], BF16, stack=ph)
        vnas = cx.sb("vnas", [128, NA_T, 65], BF16, stack=ph)
        nabs = cx.sb("nabs", [128, 5, 7 * 128], stack=ph)
        tl = cx.sb("tl", [128, 7 * 128], stack=ph)
        pna = cx.sb("pna", [128, 9 * 128], BF16, stack=ph)
        sps = [cx.ps(f"sps{i}", [128, 2, 512], stack=ph) for i in range(2)]
        ops = [cx.ps(f"ops{i}", [128, 512], stack=ph) for i in range(2)]
        pbc = cx.ps("pbc", [128, 512], stack=ph)
        cnt = {"pair": 0, "blk": 0, "ob": 0}

        def finalize(opsi, nq, kind, orow, tok0):
            okey = ("ops", opsi)
            kb.op("act", lambda e: e.copy(out=osb[:, 0:nq], in_=ops[opsi][0:65, 0:nq]), reads=[okey], writes=["osb"])
            kb.op("pe", lambda e: e.matmul(pbc[0:64, 0:nq], lhsT=sel65[0:65, :], rhs=osb[0:65, 0:nq], start=True, stop=True),
                  reads=["osb", "sel65"], writes=["pbc"])
            kb.op("dve", lambda e: e.reciprocal(out=rec[:, 0:nq], in_=pbc[0:64, 0:nq]), reads=["pbc"], writes=["rec"])
            if kind == "diff0":
                kb.op("dve", lambda e: e.tensor_tensor(out=a1[:, tok0:tok0 + nq], in0=osb[0:64, 0:nq], in1=rec[:, 0:nq], op=ALU.mult),
                      reads=["osb", "rec"], writes=[("a1", tok0)])
                return
            oi = cnt["ob"] % 2
            cnt["ob"] += 1
            if kind == "plain":
                kb.op("dve", lambda e: e.tensor_tensor(out=ob[oi][:, 0:nq], in0=osb[0:64, 0:nq], in1=rec[:, 0:nq], op=ALU.mult),
                      reads=["osb", "rec"], writes=[("ob", oi)])
            else:
                kb.op("dve", lambda e: e.tensor_tensor(out=dd[:, 0:nq], in0=osb[0:64, 0:nq], in1=rec[:, 0:nq], op=ALU.mult),
                      reads=["osb", "rec"], writes=["dd"])
                kb.op("dve", lambda e: e.scalar_tensor_tensor(out=dd[:, 0:nq], in0=dd[:, 0:nq], scalar=neglam[:, 0:1], in1=a1[:, tok0:tok0 + nq],
                                                              op0=ALU.mult, op1=ALU.add),
                      reads=["dd", "neglam", ("a1", tok0)], writes=["dd"])
                kb.op("pool", lambda e: e.tensor_tensor(out=sqd[:, 0:nq], in0=dd[:, 0:nq], in1=dd[:, 0:nq], op=ALU.mult),
                      reads=["dd"], writes=["sqd"])
                kb.op("pe", lambda e: e.matmul(pbc[0:64, 0:nq], lhsT=ones64[:, :], rhs=sqd[:, 0:nq], start=True, stop=True),
                      reads=["sqd", "ones64"], writes=["pbc"])
                kb.op("act", lambda e: e.activation(out=rsd[:, 0:nq], in_=pbc[0:64, 0:nq], func=AF.Sqrt, scale=1.0 / 64, bias=eps[0:64, 0:1]),
                      reads=["pbc", "eps"], writes=["rsd"])
                kb.op("dve", lambda e: e.reciprocal(out=rsd[:, 0:nq], in_=rsd[:, 0:nq]), reads=["rsd"], writes=["rsd"])
                kb.op("dve", lambda e: e.scalar_tensor_tensor(out=ob[oi][:, 0:nq], in0=dd[:, 0:nq], scalar=gs_sb[:, 0:1], in1=rsd[:, 0:nq],
                                                              op0=ALU.mult, op1=ALU.mult),
                      reads=["dd", "gs_sb", "rsd"], writes=[("ob", oi)])
            kb.dma("sp" if oi == 0 else "pool", oT_d[orow:orow + 64, tok0:tok0 + nq], ob[oi][:, 0:nq],
                   reads=[("ob", oi)], writes=["oT_d"], slot=("obo", oi))

        def attend(kt_ap_fn, kkey, v_ap_fn, vkey, q_ap, qkey, dk, ktiles, nq, scale, kind, orow, tok0):
            opsi = cnt["blk"] % 2
            cnt["blk"] += 1
            pairs = [(ktiles[2 * i], ktiles[2 * i + 1]) for i in range(len(ktiles) // 2)]
            npair = len(pairs)
            base = cnt["pair"]
            cnt["pair"] += npair

            def emit_S(p):
                si = (base + p) % 2
                for j, kt in enumerate(pairs[p]):
                    kb.op("pe", lambda e: e.matmul(sps[si][:, j, 0:nq], lhsT=kt_ap_fn(kt), rhs=q_ap, start=True, stop=True),
                          reads=list(kkey) + list(qkey), writes=[("sps", si, j)])

            emit_S(0)
            for p in range(npair):
                si = (base + p) % 2
                if p + 1 < npair:
                    emit_S(p + 1)
                for j in range(2):
                    kb.op("act", lambda e: e.activation(out=pT[si][:, j, 0:nq], in_=sps[si][:, j, 0:nq], func=AF.Exp, scale=float(scale)),
                          reads=[("sps", si, j)], writes=[("pT", si, j)])
                for j, kt in enumerate(pairs[p]):
                    kb.op("pe", lambda e: e.matmul(ops[opsi][0:65, 0:nq], lhsT=v_ap_fn(kt), rhs=pT[si][:, j, 0:nq],
                                                   start=(p == 0 and j == 0), stop=(p == npair - 1 and j == 1)),
                          reads=list(vkey) + [("pT", si, j)], writes=[("ops", opsi)])
            finalize(opsi, nq, kind, orow, tok0)

        heads = _full_heads()
        vA_v = vA.rearrange("t p w -> p t w")

        def load_head(hi):
            hd = heads[hi]
            s = hi % 2
            p0 = 0
            for i, (r0, n) in enumerate(hd["k"]):
                kb.dma("sp", ksb[s][p0:p0 + n, :], kT[r0:r0 + n, :], writes=[("ksb", s, i)], slot=("ksb", s))
                p0 += n
            if len(hd["k"]) == 1:
                kb.last_w[("ksb", s, 1)] = kb.last_w[("ksb", s, 0)]
                kb.readers[("ksb", s, 1)] = []
            p0 = 0
            for (r0, n) in hd["q"]:
                kb.dma("sp", qsb[s][p0:p0 + n, :], qT[r0:r0 + n, :], writes=[("qsb", s)], slot=("qsb", s))
                p0 += n
            vh = hd["vh"]
            for part in range(5):
                t0, t1 = part * 26, (part + 1) * 26
                kb.dma("pool", vsb[s][:, t0:t1, :], vA_v[:, t0:t1, vh * 65:(vh + 1) * 65], writes=[("vsb", s, part)], slot=("vsb", s))

        if "f" not in parts:
            heads = []
        heads = heads[:nheads]
        if heads:
            load_head(0)
        all_kt = list(range(NKT))
        for hi, hd in enumerate(heads):
            s = hi % 2
            if hi + 1 < len(heads):
                load_head(hi + 1)
            dk = hd["dk"]
            for qb in range(NT // 512):
                attend(lambda kt: ksb[s][0:dk, kt * 128:(kt + 1) * 128], [("ksb", s, 0), ("ksb", s, 1)],
                       lambda kt: vsb[s][:, kt, :], [("vsb", s, i) for i in range(5)],
                       qsb[s][0:dk, qb * 512:(qb + 1) * 512], [("qsb", s)], dk, all_kt, 512, hd["scale"], hd["kind"],
                       hd["orow"], qb * 512)
            if need_ctx:
                attend(lambda kt: ksb[s][0:dk, kt * 128:(kt + 1) * 128], [("ksb", s, 0), ("ksb", s, 1)],
                       lambda kt: vsb[s][:, kt, :], [("vsb", s, i) for i in range(5)],
                       qsb[s][0:dk, NT:NTOK], [("qsb", s)], dk, [0, 1], NCTX, hd["scale"], hd["kind"], hd["orow"], NT)

        vna_v = vna.rearrange("t p w -> p t w")
        sna = [sps[i][:].rearrange("p a b -> p (a b)") for i in range(2)]
        for h in range(4 if "n" in parts else 0):
            kb.dma("sp", knas[:, :], kna[64 * h:64 * (h + 1), :], writes=["knas"], slot=("knas",))
            kb.dma("sp", qsb[0][0:64, :], qT[64 * h:64 * (h + 1), :], writes=[("qsb", 0)], slot=("qsb", 0))
            kb.dma("pool", vnas[:], vna_v[:, :, h * 65:(h + 1) * 65], writes=["vnas"], slot=("vnas",))
            kb.dma("pool", nabs[:].rearrange("p a b -> p (a b)"), nab[h, :, :], writes=["nabs"], slot=("nabs",))
            for i in range(NT // 128):
                slot_i = {0: 0, 1: 1, 30: 3, 31: 4}.get(i, 2)
                for a in range(7):
                    ktile = 2 + i + a
                    kb.op("pe", lambda e: e.matmul(sna[0][:, a * 128:(a + 1) * 128], lhsT=knas[0:64, ktile * 128:(ktile + 1) * 128],
                                                   rhs=qsb[0][0:64, i * 128:(i + 1) * 128], start=True, stop=True),
                          reads=["knas", ("qsb", 0)], writes=[("sps", 0, 0), ("sps", 0, 1)])
                for a in range(2):
                    kb.op("pe", lambda e: e.matmul(sna[1][:, a * 128:(a + 1) * 128], lhsT=knas[0:64, a * 128:(a + 1) * 128],
                                                   rhs=qsb[0][0:64, i * 128:(i + 1) * 128], start=True, stop=True),
                          reads=["knas", ("qsb", 0)], writes=[("sps", 1, 0)])
                for (c0, c1) in ((0, 512), (512, 896)):
                    kb.op("dve", lambda e: e.scalar_tensor_tensor(out=tl[:, c0:c1], in0=sna[0][:, c0:c1], scalar=0.125,
                                                                  in1=nabs[:, slot_i, c0:c1], op0=ALU.mult, op1=ALU.add),
                          reads=[("sps", 0, 0), ("sps", 0, 1), "nabs"], writes=[("tl", c0)])
                kb.op("act", lambda e: e.activation(out=pna[:, 0:896], in_=tl[:], func=AF.Exp), reads=[("tl", 0), ("tl", 512)], writes=[("pna", 0)])
                kb.op("act", lambda e: e.activation(out=pna[:, 896:1152], in_=sna[1][:, 0:256], func=AF.Exp, scale=0.125),
                      reads=[("sps", 1, 0)], writes=[("pna", 1)])
                col = (i % 4) * 128
                for a in range(9):
                    ktile = (2 + i + a) if a < 7 else (a - 7)
                    kb.op("pe", lambda e: e.matmul(ops[0][0:65, col:col + 128], lhsT=vnas[:, ktile, :], rhs=pna[:, a * 128:(a + 1) * 128],
                                                   start=(a == 0), stop=(a == 8)),
                          reads=["vnas", ("pna", 0), ("pna", 1)], writes=[("ops", 0)])
                if i % 4 == 3:
                    finalize(0, 512, "plain", 64 * h, (i // 4) * 512)
            if need_ctx:
                attend(lambda kt: knas[0:64, kt * 128:(kt + 1) * 128], ["knas"], lambda kt: vnas[:, kt, :], ["vnas"],
                       qsb[0][0:64, NT:NTOK], [("qsb", 0)], 64, [0, 1], NCTX, 0.125, "plain", 64 * h, NT)
        cx.barrier()

    with contextlib.ExitStack() as ph:
        wout_bf = cx.sb("wout_bf", [128, 8, D], BF16, stack=ph)
        wst = [cx.sb(f"wst{i}", [128, D], stack=ph) for i in range(2)]
        G1 = cx.sb("G1", [128, 2, D], stack=ph)
        A2 = cx.sb("A2", [128, 2, D], stack=ph)
        B2 = cx.sb("B2", [128, 2, D], stack=ph)
        gb = cx.sb("gb", [128, D], stack=ph)
        wr_sb = cx.sb("wr_sb", [128, 8, NE], stack=ph)
        oT = [cx.sb(f"oT{i}", [128, 8, 128], BF16, stack=ph) for i in range(2)]
        xt = [cx.sb(f"xt{i}", [128, D], stack=ph) for i in range(2)]
        x1 = [cx.sb(f"x1{i}", [128, D], stack=ph) for i in range(2)]
        tmp = cx.sb("tmp", [128, D], stack=ph)
        junk = cx.sb("junk", [128, D], stack=ph)
        h2 = cx.sb("h2", [128, D], stack=ph)
        h2Tf = cx.sb("h2Tf", [128, D], stack=ph)
        sH = [cx.sb(f"sH{i}", [128, 8, 512], BF16, stack=ph) for i in range(2)]
        st_ss = cx.sb("st_ss", [128, 1], stack=ph)
        st_rstd = cx.sb("st_rstd", [128, 1], stack=ph)
        lg = cx.sb("lg", [128, NE], stack=ph)
        mx = cx.sb("mx", [128, 1], stack=ph)
        sm = cx.sb("sm", [128, 1], stack=ph)
        af = [cx.sb(f"af{i}", [128, NE], stack=ph) for i in range(2)]
        py = cx.ps("py", [128, 2, 512], stack=ph)
        ptr = cx.ps("ptr", [128, D], stack=ph)
        plog = cx.ps("plog", [128, 512], stack=ph)
        for c in range(8):
            s = c % 2
            kb.dma("sp" if s == 0 else "pool", wst[s][:], wout[c * 128:(c + 1) * 128, :], writes=[("wst", s)], slot=("wstA", s))
            kb.op("dve" if s == 0 else "pool", lambda e: e.tensor_copy(out=wout_bf[:, c, :], in_=wst[s][:]), reads=[("wst", s)], writes=["wout_bf"])
        kb.dma("sp", wr_sb[:], wr.rearrange("(c p) e -> p c e", p=128), writes=["wr_sb"])
        kb.dma("sp", gb[:], gffn[0:1, :].broadcast_to([128, D]), writes=["gb"])
        for m in range(2):
            kb.dma("sp", G1[:, m, :], modv[m:m + 1, 2 * D:3 * D].broadcast_to([128, D]), writes=[("G1", m)])
            kb.dma("pool", B2[:, m, :], modv[m:m + 1, 3 * D:4 * D].broadcast_to([128, D]), writes=[("B2", m)])
            kb.dma("sp", tmp[:], modv[m:m + 1, 4 * D:5 * D].broadcast_to([128, D]), writes=["tmp"])
            kb.op("dve", lambda e: e.scalar_tensor_tensor(out=A2[:, m, :], in0=tmp[:], scalar=1.0, in1=gb[:], op0=ALU.add, op1=ALU.mult),
                  reads=["tmp", "gb"], writes=[("A2", m)])
        oT_v = oT_d.rearrange("(c p) t -> p c t", p=128)
        h2T_v = h2T_o.rearrange("(c p) t -> p c t", p=128)
        for t in range(NQT if "p" in parts else 0):
            m = 0 if t < NT // 128 else 1
            s = t % 2
            grp = t // 4
            gs = grp % 2
            tin = t % 4
            dq = "sp" if s == 0 else "pool"
            kb.dma(dq, oT[s][:], oT_v[:, :, t * 128:(t + 1) * 128], reads=["oT_d"], writes=[("oT", s)], slot=("oT", s))
            kb.dma(dq, xt[s][:], xin[t * 128:(t + 1) * 128, :], writes=[("xt", s)], slot=("xtA", s))
            for cb in range(2):
                for c in range(8):
                    kb.op("pe", lambda e: e.matmul(py[:, cb, :], lhsT=oT[s][:, c, :], rhs=wout_bf[:, c, cb * 512:(cb + 1) * 512],
                                                   start=(c == 0), stop=(c == 7)),
                          reads=[("oT", s), "wout_bf"], writes=[("py", cb)])
            for cb in range(2):
                kb.op("dve", lambda e: e.tensor_tensor(out=tmp[:, cb * 512:(cb + 1) * 512], in0=py[:, cb, :], in1=G1[:, m, cb * 512:(cb + 1) * 512], op=ALU.mult),
                      reads=[("py", cb), ("G1", m)], writes=[("tmp", cb)])
            kb.op("pool", lambda e: e.tensor_tensor(out=x1[s][:], in0=tmp[:], in1=xt[s][:], op=ALU.add),
                  reads=[("tmp", 0), ("tmp", 1), ("xt", s)], writes=[("x1", s), "tmp"])
            kb.dma(dq, x1_o[t * 128:(t + 1) * 128, :], x1[s][:], reads=[("x1", s)], writes=["x1_d"], slot=("x1o", s))
            kb.op("act", lambda e: e.activation(out=junk[:], in_=x1[s][:], func=AF.Square, accum_out=st_ss[:]),
                  reads=[("x1", s)], writes=["junk", "st_ss"])
            rstd_from_ss(cx, st_ss[:], st_rstd[:], D, eps[:, 0:1], ["st_ss"], "st_rstd")
            kb.op("dve", lambda e: e.scalar_tensor_tensor(out=tmp[:], in0=x1[s][:], scalar=st_rstd[:, 0:1], in1=A2[:, m, :],
                                                          op0=ALU.mult, op1=ALU.mult),
                  reads=[("x1", s), "st_rstd", ("A2", m)], writes=["tmp", ("tmp", 0), ("tmp", 1)])
            kb.op("pool", lambda e: e.tensor_tensor(out=h2[:], in0=tmp[:], in1=B2[:, m, :], op=ALU.add),
                  reads=["tmp", ("B2", m)], writes=["h2"])
            for c in range(8):
                kb.op("pe", lambda e: e.transpose(out=ptr[:, c * 128:(c + 1) * 128], in_=h2[:, c * 128:(c + 1) * 128], identity=identf[:]),
                      reads=["h2", "ident_f"], writes=[("ptr", c)])
            for hb_ in range(2):
                kb.op("act", lambda e: e.copy(out=h2Tf[:, hb_ * 512:(hb_ + 1) * 512], in_=ptr[:, hb_ * 512:(hb_ + 1) * 512]),
                      reads=[("ptr", c) for c in range(4 * hb_, 4 * hb_ + 4)], writes=[("h2Tf", hb_)])
                kb.op("dve", lambda e: e.tensor_copy(out=sH[gs][:, 4 * hb_:4 * hb_ + 4, tin * 128:(tin + 1) * 128],
                                                     in_=h2Tf[:, hb_ * 512:(hb_ + 1) * 512].rearrange("p (c w) -> p c w", w=128)),
                      reads=[("h2Tf", hb_)], writes=[("sH", gs)])
            for c in range(8):
                kb.op("pe", lambda e: e.matmul(plog[:, 0:NE], lhsT=h2Tf[:, c * 128:(c + 1) * 128], rhs=wr_sb[:, c, :], start=(c == 0), stop=(c == 7)),
                      reads=[("h2Tf", 0), ("h2Tf", 1), "wr_sb"], writes=["plog"])
            kb.op("dve", lambda e: e.tensor_reduce(out=mx[:], in_=plog[:, 0:NE], axis=AX.X, op=ALU.max), reads=["plog"], writes=["mx"])
            kb.op("dve", lambda e: e.tensor_scalar(out=mx[:], in0=mx[:], scalar1=-1.0, scalar2=None, op0=ALU.mult), reads=["mx"], writes=["mx"])
            kb.op("act", lambda e: e.activation(out=lg[:], in_=plog[:, 0:NE], func=AF.Exp, bias=mx[:, 0:1], accum_out=sm[:]),
                  reads=["plog", "mx"], writes=["lg", "sm"])
            kb.op("dve", lambda e: e.reciprocal(out=sm[:], in_=sm[:]), reads=["sm"], writes=["sm"])
            kb.op("dve", lambda e: e.tensor_scalar(out=af[s][:], in0=lg[:], scalar1=sm[:, 0:1], scalar2=None, op0=ALU.mult),
                  reads=["lg", "sm"], writes=[("af", s)])
            kb.dma(dq, aff_o[t * 128:(t + 1) * 128, :], af[s][:], reads=[("af", s)], writes=["aff_d"], slot=("afo", s))
            if tin == 3 or t == NQT - 1:
                ntk = (tin + 1) * 128
                kb.dma("sp", h2T_v[:, :, grp * 512:grp * 512 + ntk], sH[gs][:, :, 0:ntk], reads=[("sH", gs)], writes=["h2T_d"], slot=("sHo", gs))
        cx.barrier()
    return cx.finish()


def _na_bias_tables(rpb, r):
    H = 4
    rows = SEQ // 64
    out = np.full((H, 5, 128, 7, 128), -30000.0, np.float32)
    kp = np.arange(128)
    qp = np.arange(128)
    for si, i_loc in enumerate((0, 1, 5, 30, 31)):
        j = 32 * r + i_loc
        qr = 2 * j + qp // 64
        qc = qp % 64
        r0 = np.clip(qr - 4, 0, rows - 8)
        c0 = np.clip(qc - 8, 0, 64 - 16)
        for t in range(7):
            g = j - 3 + t
            if g < 0 or g >= 128:
                continue
            kr = 2 * g + kp // 64
            kc = kp % 64
            valid = ((kr[:, None] >= r0[None, :]) & (kr[:, None] < r0[None, :] + 8) &
                     (kc[:, None] >= c0[None, :]) & (kc[:, None] < c0[None, :] + 16))
            ri = np.clip(kr[:, None] - qr[None, :] + 7, 0, 14)
            ci = np.clip(kc[:, None] - qc[None, :] + 15, 0, 30)
            for h in range(H):
                vals = rpb[h][ri, ci]
                out[h, si, :, t, :] = np.where(valid, vals, np.float32(-30000.0))
    return np.ascontiguousarray(out.transpose(0, 2, 1, 3, 4).reshape(H, 128, 5 * 7 * 128))


def run_A(inp, l, resP, x_cur, xc_cur, **kw):
    nc = _get_nc("A", build_A, l) if not kw else build_A(l, **kw)
    in_maps = []
    zk = None
    for c in range(NCORE):
        b, r = c // 4, c % 4
        grp = [resP[4 * b + rr] for rr in range(4)]
        kT_all = np.concatenate([resP[c]["kT"][:, NT:NTOK]] + [g["kT"][:, 0:NT] for g in grp], axis=1)
        v_all = np.concatenate([resP[c]["v"][32:34]] + [g["v"][0:32] for g in grp], axis=0)
        kna = np.zeros((256, NA_T * 128), kT_all.dtype)
        vna = np.zeros((NA_T, 128, 4 * 65), v_all.dtype)
        kna[:, 0:256] = kT_all[0:256, 0:256]
        vna[0:2] = v_all[0:2, :, 0:260]
        for i in range(32 + 2 * NA_HALO):
            g = 32 * r + i - NA_HALO
            if 0 <= g < 128:
                kna[:, (2 + i) * 128:(3 + i) * 128] = kT_all[0:256, 256 + g * 128:256 + (g + 1) * 128]
                vna[2 + i] = v_all[2 + g, :, 0:260]
        in_maps.append({
            "qT": resP[c]["qT"], "kT": np.ascontiguousarray(kT_all), "v": np.ascontiguousarray(v_all),
            "kna": kna, "vna": vna, "nab": _na_bias_tables(np.asarray(inp["na_rpb"][l], np.float32), r),
            "xin": np.ascontiguousarray(np.concatenate([x_cur[b, r * NT:(r + 1) * NT], xc_cur[b]], axis=0)),
            "wout": np.ascontiguousarray(inp["w_out"][l]),
            "modv": resP[c]["modv"],
            "gffn": np.ascontiguousarray(inp["g_ffn"][l][None, :]),
            "wr": np.ascontiguousarray(inp["w_router"][l]),
            "gsub": np.ascontiguousarray(inp["g_diff_sub"][l].reshape(64, 1)),
            "lamp": np.ascontiguousarray(inp["diff_lambda"][l].reshape(1, 128)),
        })
    res = run_bass_kernel_spmd(nc, in_maps, core_ids=list(range(NCORE)))
    return res.results, in_maps


def _e_parts(l):
    return [(0, 12), (12, 12), (24, 10)] if l == 0 else [(0, 11), (11, 11), (22, 10)]


def build_E(l, n_exp=NE, n_jp=DFF // 256):
    need_ctx = (l == 0)
    NQ = NTOK if need_ctx else NT
    cx = Ctx()
    nc, kb = cx.nc, cx.kb
    h2T = cx.din("h2T", [D, NQ], BF16)
    affA = cx.din("affA", [NE, SEQ])
    affC = cx.din("affC", [NE, NCTX])
    affO = cx.din("affO", [NE, NQ])
    x1 = cx.din("x1", [NQ, D])
    modv = cx.din("modv", [2, 6 * D])
    wg = cx.din("wg", [NE, D, DFF])
    wu = cx.din("wu", [NE, D, DFF])
    wd = cx.din("wd", [NE, DFF, D])
    x2_o = cx.dout("x2", [NQ, D])

    identf, identb = make_ident(cx)
    gsel = cx.sb("gsel", [NE, NQ])
    thr = cx.sb("thr", [NE, 2])

    with contextlib.ExitStack() as ph:
        aall = cx.sb("aall", [NE, SEQ], stack=ph)
        jk = cx.sb("jk", [NE, SEQ], BF16, stack=ph)
        actx = cx.sb("actx", [NE, NCTX], stack=ph)
        aown = cx.sb("aown", [NE, NQ], stack=ph)
        msk = cx.sb("msk", [NE, NQ], stack=ph)
        lo = cx.sb("lo", [NE, 1], stack=ph)
        hi = cx.sb("hi", [NE, 1], stack=ph)
        mid = cx.sb("mid", [NE, 1], stack=ph)
        cn = cx.sb("cn", [NE, 1], stack=ph)
        ge = cx.sb("ge", [NE, 1], stack=ph)
        d1 = cx.sb("d1", [NE, 1], stack=ph)
        d2 = cx.sb("d2", [NE, 1], stack=ph)
        for q in range(4):
            kb.dma("sp" if q % 2 == 0 else "pool", aall[:, q * 4096:(q + 1) * 4096], affA[:, q * 4096:(q + 1) * 4096], writes=[("aall", q)])
        kb.dma("sp", actx[:], affC[:, :], writes=["actx"])
        kb.dma("sp", aown[:], affO[:, :], writes=["aown"])
        AALL = [("aall", q) for q in range(4)]

        def bisect(data_ap, n, cap, rkeys, col):
            kb.op("dve", lambda e: e.memset(lo[:], 0.0), writes=["lo"])
            kb.op("dve", lambda e: e.memset(hi[:], 1.0), writes=["hi"])
            for it in range(40):
                kb.op("dve", lambda e: e.tensor_tensor(out=mid[:], in0=lo[:], in1=hi[:], op=ALU.add), reads=["lo", "hi"], writes=["mid"])
                kb.op("dve", lambda e: e.tensor_scalar(out=mid[:], in0=mid[:], scalar1=0.5, scalar2=None, op0=ALU.mult), reads=["mid"], writes=["mid"])
                kb.op("dve", lambda e: e.tensor_scalar(out=jk[:, 0:n], in0=data_ap, scalar1=mid[:, 0:1], scalar2=None, op0=ALU.is_gt, op1=ALU.add,
                                                       accum_out=cn[:, 0:1]), reads=list(rkeys) + ["mid"], writes=["jk", "cn"])
                kb.op("dve", lambda e: e.tensor_scalar(out=ge[:], in0=cn[:], scalar1=float(cap) - 0.5, scalar2=None, op0=ALU.is_gt), reads=["cn"], writes=["ge"])
                kb.op("dve", lambda e: e.tensor_tensor(out=d1[:], in0=mid[:], in1=lo[:], op=ALU.subtract), reads=["mid", "lo"], writes=["d1"])
                kb.op("dve", lambda e: e.tensor_tensor(out=d2[:], in0=hi[:], in1=mid[:], op=ALU.subtract), reads=["mid", "hi"], writes=["d2"])
                kb.op("dve", lambda e: e.scalar_tensor_tensor(out=lo[:], in0=d1[:], scalar=ge[:, 0:1], in1=lo[:], op0=ALU.mult, op1=ALU.add),
                      reads=["d1", "ge", "lo"], writes=["lo"])
                kb.op("dve", lambda e: e.scalar_tensor_tensor(out=hi[:], in0=d2[:], scalar=ge[:, 0:1], in1=mid[:], op0=ALU.mult, op1=ALU.add),
                      reads=["d2", "ge", "mid"], writes=["hi"])
            kb.op("dve", lambda e: e.tensor_copy(out=thr[:, col:col + 1], in_=lo[:]), reads=["lo"], writes=[("thr", col)])

        bisect(aall[:, :], SEQ, 2 * SEQ // NE, AALL, 0)
        if need_ctx:
            bisect(actx[:, :], NCTX, 2 * NCTX // NE, ["actx"], 1)
        segs = [(0, NT, 0)] + ([(NT, NTOK, 1)] if need_ctx else [])
        for (c0, c1, col) in segs:
            kb.op("dve", lambda e: e.tensor_scalar(out=msk[:, c0:c1], in0=aown[:, c0:c1], scalar1=thr[:, col:col + 1], scalar2=None, op0=ALU.is_gt),
                  reads=["aown", ("thr", col)], writes=[("msk", col)])
            kb.op("dve", lambda e: e.tensor_tensor(out=gsel[:, c0:c1], in0=msk[:, c0:c1], in1=aown[:, c0:c1], op=ALU.mult),
                  reads=[("msk", col), "aown"], writes=["gsel"])
        cx.barrier()

    MAXT = 12 * 128
    oh = cx.sb("oh", [NE, NE, 128])
    kb.op("pool", lambda e: e.memset(oh[:], 1.0), writes=["oh"])
    kb.op("pool", lambda e: e.affine_select(out=oh[:], in_=oh[:], pattern=[[-1, NE], [0, 128]], compare_op=ALU.is_equal, fill=0.0,
                                            base=0, channel_multiplier=1), reads=["oh"], writes=["oh"])
    G2 = cx.sb("G2", [128, 2, D])
    for m in range(2):
        kb.dma("sp", G2[:, m, :], modv[m:m + 1, 5 * D:6 * D].broadcast_to([128, D]), writes=[("G2", m)])
    hsb = cx.sb("hsb", [128, 8, MAXT], BF16)
    yacc = cx.sb("yacc", [128, 8, MAXT])
    gbc = cx.sb("gbc", [128, MAXT])
    stg = cx.sb("stg", [128, 8, 256])
    stu = cx.sb("stu", [128, 8, 256])
    std = cx.sb("std", [128, 2, D])
    wgb = [cx.sb(f"wgb{i}", [128, 8, 256], BF16) for i in range(2)]
    wub = [cx.sb(f"wub{i}", [128, 8, 256], BF16) for i in range(2)]
    wdb = [cx.sb(f"wdb{i}", [128, 2, D], BF16) for i in range(2)]
    sg = cx.sb("sg", [128, 2, 512])
    ug = cx.sb("ug", [128, 2, 512])
    act = [cx.sb(f"act{i}", [128, 2, 512], BF16) for i in range(2)]
    xt = cx.sb("xt", [128, D])
    tmp = cx.sb("tmp", [128, D])
    x2 = [cx.sb(f"x2{i}", [128, D]) for i in range(2)]
    gps = cx.ps("gps", [128, 2, 512])
    ups = cx.ps("ups", [128, 2, 512])
    yps = [cx.ps(f"yps{i}", [128, 512]) for i in range(3)]
    pgb = cx.ps("pgb", [128, 512])
    h2T_v = h2T.rearrange("(c p) t -> p c t", p=128)
    cnt = {"w": 0, "y": 0, "a": 0}

    for (t0, ntl) in _e_parts(l):
        tok0, ntok = t0 * 128, ntl * 128
        blocks = [(b, min(512, ntok - b)) for b in range(0, ntok, 512)]
        kb.dma("sp", hsb[:, :, 0:ntok], h2T_v[:, :, tok0:tok0 + ntok], writes=["hsb"], slot=("hsb",))
        kb.op("pool", lambda e: e.memset(yacc[:], 0.0), writes=[("yacc", dc) for dc in range(8)])
        for ex in range(n_exp):
            for (b0, n) in blocks:
                kb.op("pe", lambda e: e.matmul(pgb[:, 0:n], lhsT=oh[:, ex, :], rhs=gsel[:, tok0 + b0:tok0 + b0 + n], start=True, stop=True),
                      reads=["oh", "gsel"], writes=["pgb"])
                kb.op("act", lambda e: e.copy(out=gbc[:, b0:b0 + n], in_=pgb[:, 0:n]), reads=["pgb"], writes=["gbc"])
            for jp in range(n_jp):
                ws = cnt["w"] % 2
                cnt["w"] += 1
                f0 = jp * 256
                kb.dma("sp", stg[:], wg[ex].rearrange("(c p) f -> p c f", p=128)[:, :, f0:f0 + 256], writes=["stg"], slot=("stg",))
                kb.dma("sp", stu[:], wu[ex].rearrange("(c p) f -> p c f", p=128)[:, :, f0:f0 + 256], writes=["stu"], slot=("stu",))
                kb.dma("pool", std[:], wd[ex, f0:f0 + 256, :].rearrange("(j p) d -> p j d", p=128), writes=["std"], slot=("std",))
                kb.op("pool", lambda e: e.tensor_copy(out=wgb[ws][:], in_=stg[:]), reads=["stg"], writes=[("wgb", ws)])
                kb.op("pool", lambda e: e.tensor_copy(out=wub[ws][:], in_=stu[:]), reads=["stu"], writes=[("wub", ws)])
                kb.op("pool", lambda e: e.tensor_copy(out=wdb[ws][:], in_=std[:]), reads=["std"], writes=[("wdb", ws)])
                for (b0, n) in blocks:
                    ai = cnt["a"] % 2
                    cnt["a"] += 1
                    for jj in range(2):
                        for c in range(8):
                            kb.op("pe", lambda e: e.matmul(gps[:, jj, 0:n], lhsT=wgb[ws][:, c, jj * 128:(jj + 1) * 128], rhs=hsb[:, c, b0:b0 + n],
                                                           start=(c == 0), stop=(c == 7)),
                                  reads=[("wgb", ws), "hsb"], writes=[("gps", jj)])
                        for c in range(8):
                            kb.op("pe", lambda e: e.matmul(ups[:, jj, 0:n], lhsT=wub[ws][:, c, jj * 128:(jj + 1) * 128], rhs=hsb[:, c, b0:b0 + n],
                                                           start=(c == 0), stop=(c == 7)),
                                  reads=[("wub", ws), "hsb"], writes=[("ups", jj)])
                    for jj in range(2):
                        kb.op("act", lambda e: e.activation(out=sg[:, jj, 0:n], in_=gps[:, jj, 0:n], func=AF.Silu), reads=[("gps", jj)], writes=[("sg", jj)])
                        kb.op("dve", lambda e: e.tensor_tensor(out=ug[:, jj, 0:n], in0=ups[:, jj, 0:n], in1=gbc[:, b0:b0 + n], op=ALU.mult),
                              reads=[("ups", jj), "gbc"], writes=[("ug", jj)])
                        kb.op("pool", lambda e: e.tensor_tensor(out=act[ai][:, jj, 0:n], in0=sg[:, jj, 0:n], in1=ug[:, jj, 0:n], op=ALU.mult),
                              reads=[("sg", jj), ("ug", jj)], writes=[("act", ai, jj)])
                    for dc in range(8):
                        yi = cnt["y"] % 3
                        cnt["y"] += 1
                        for jj in range(2):
                            kb.op("pe", lambda e: e.matmul(yps[yi][:, 0:n], lhsT=wdb[ws][:, jj, dc * 128:(dc + 1) * 128], rhs=act[ai][:, jj, 0:n],
                                                           start=(jj == 0), stop=(jj == 1)),
                                  reads=[("wdb", ws), ("act", ai, jj)], writes=[("yps", yi)])
                        kb.op("dve", lambda e: e.tensor_tensor(out=yacc[:, dc, b0:b0 + n], in0=yps[yi][:, 0:n], in1=yacc[:, dc, b0:b0 + n], op=ALU.add),
                              reads=[("yps", yi), ("yacc", dc)], writes=[("yacc", dc)])
        for tt in range(ntl):
            t = t0 + tt
            m = 0 if t < NT // 128 else 1
            s = tt % 2
            kb.dma("sp", xt[:], x1[t * 128:(t + 1) * 128, :], writes=["xt"], slot=("xtE",))
            for dc in range(8):
                kb.op("pe", lambda e: e.transpose(out=gps[:, dc // 4, (dc % 4) * 128:(dc % 4 + 1) * 128], in_=yacc[:, dc, tt * 128:(tt + 1) * 128],
                                                  identity=identf[:]),
                      reads=[("yacc", dc), "ident_f"], writes=[("gps", dc // 4)])
            for hb_ in range(2):
                kb.op("dve", lambda e: e.tensor_tensor(out=tmp[:, hb_ * 512:(hb_ + 1) * 512], in0=gps[:, hb_, :], in1=G2[:, m, hb_ * 512:(hb_ + 1) * 512], op=ALU.mult),
                      reads=[("gps", hb_), ("G2", m)], writes=[("tmp", hb_)])
            kb.op("pool", lambda e: e.tensor_tensor(out=x2[s][:], in0=tmp[:], in1=xt[:], op=ALU.add),
                  reads=[("tmp", 0), ("tmp", 1), "xt"], writes=[("x2", s)])
            kb.dma("sp", x2_o[t * 128:(t + 1) * 128, :], x2[s][:], reads=[("x2", s)], writes=["x2_d"], slot=("x2o", s))
    return cx.finish()


def run_E(inp, l, resA, modvs, **kw):
    nc = _get_nc("E", build_E, l) if not kw else build_E(l, **kw)
    need_ctx = (l == 0)
    in_maps = []
    wg = np.ascontiguousarray(inp["w_gate"][l])
    wu = np.ascontiguousarray(inp["w_up"][l])
    wd = np.ascontiguousarray(inp["w_down"][l])
    for c in range(NCORE):
        b, r = c // 4, c % 4
        affA = np.ascontiguousarray(np.concatenate([resA[4 * b + rr]["aff"][0:NT] for rr in range(4)], axis=0).T)
        affO = np.ascontiguousarray(resA[c]["aff"].T)
        affC = np.ascontiguousarray(resA[c]["aff"][NT:NTOK].T) if need_ctx else np.zeros((NE, NCTX), np.float32)
        in_maps.append({"h2T": resA[c]["h2T"], "affA": affA, "affC": affC, "affO": affO, "x1": resA[c]["x1"],
                        "modv": modvs[c], "wg": wg, "wu": wu, "wd": wd})
    res = run_bass_kernel_spmd(nc, in_maps, core_ids=list(range(NCORE)))
    return res.results, in_maps


NEC = 4
PART_T = 12


def build_E2(l):
    need_ctx = (l == 0)
    NB = NTOK if need_ctx else NT
    NBT = SEQ + (NCTX if need_ctx else 0)
    NALL = 2 * NBT
    cx = Ctx()
    nc, kb = cx.nc, cx.kb
    h2T = cx.din("h2T", [D, NALL], BF16)
    affM = cx.din("affM", [2, NE, SEQ])
    affC = cx.din("affC", [2, NE, NCTX])
    affS = cx.din("affS", [NE, NALL])
    wg = cx.din("wg", [NEC, D, DFF])
    wu = cx.din("wu", [NEC, D, DFF])
    wd = cx.din("wd", [NEC, DFF, D])
    part_o = cx.dout("part", [NALL, D], BF16)
    gsel_d = cx.dint("gsel_d", [NE, NALL])

    identf, identb = make_ident(cx)
    thr = cx.sb("thr", [NE, 4])

    with contextlib.ExitStack() as ph:
        aall = cx.sb("aall", [NE, SEQ], stack=ph)
        jk = cx.sb("jk", [NE, SEQ], BF16, stack=ph)
        actx = cx.sb("actx", [NE, NCTX], stack=ph)
        aown = cx.sb("aown", [NE, 4096], stack=ph)
        msk = cx.sb("msk", [NE, 4096], stack=ph)
        gs_t = cx.sb("gs_t", [NE, 4096], stack=ph)
        lo = cx.sb("lo", [NE, 1], stack=ph)
        hi = cx.sb("hi", [NE, 1], stack=ph)
        mid = cx.sb("mid", [NE, 1], stack=ph)
        cn = cx.sb("cn", [NE, 1], stack=ph)
        ge = cx.sb("ge", [NE, 1], stack=ph)
        d1 = cx.sb("d1", [NE, 1], stack=ph)
        d2 = cx.sb("d2", [NE, 1], stack=ph)

        def bisect(data_ap, n, cap, rkeys, col):
            kb.op("dve", lambda e: e.memset(lo[:], 0.0), writes=["lo"])
            kb.op("dve", lambda e: e.memset(hi[:], 1.0), writes=["hi"])
            for it in range(40):
                kb.op("dve", lambda e: e.tensor_tensor(out=mid[:], in0=lo[:], in1=hi[:], op=ALU.add), reads=["lo", "hi"], writes=["mid"])
                kb.op("dve", lambda e: e.tensor_scalar(out=mid[:], in0=mid[:], scalar1=0.5, scalar2=None, op0=ALU.mult), reads=["mid"], writes=["mid"])
                kb.op("dve", lambda e: e.tensor_scalar(out=jk[:, 0:n], in0=data_ap, scalar1=mid[:, 0:1], scalar2=None, op0=ALU.is_gt, op1=ALU.add,
                                                       accum_out=cn[:, 0:1]), reads=list(rkeys) + ["mid"], writes=["jk", "cn"])
                kb.op("dve", lambda e: e.tensor_scalar(out=ge[:], in0=cn[:], scalar1=float(cap) - 0.5, scalar2=None, op0=ALU.is_gt), reads=["cn"], writes=["ge"])
                kb.op("dve", lambda e: e.tensor_tensor(out=d1[:], in0=mid[:], in1=lo[:], op=ALU.subtract), reads=["mid", "lo"], writes=["d1"])
                kb.op("dve", lambda e: e.tensor_tensor(out=d2[:], in0=hi[:], in1=mid[:], op=ALU.subtract), reads=["mid", "hi"], writes=["d2"])
                kb.op("dve", lambda e: e.scalar_tensor_tensor(out=lo[:], in0=d1[:], scalar=ge[:, 0:1], in1=lo[:], op0=ALU.mult, op1=ALU.add),
                      reads=["d1", "ge", "lo"], writes=["lo"])
                kb.op("dve", lambda e: e.scalar_tensor_tensor(out=hi[:], in0=d2[:], scalar=ge[:, 0:1], in1=mid[:], op0=ALU.mult, op1=ALU.add),
                      reads=["d2", "ge", "mid"], writes=["hi"])
            kb.op("dve", lambda e: e.tensor_copy(out=thr[:, col:col + 1], in_=lo[:]), reads=["lo"], writes=[("thr", col)])

        for b in range(2):
            for q in range(4):
                kb.dma("sp" if q % 2 == 0 else "pool", aall[:, q * 4096:(q + 1) * 4096], affM[b, :, q * 4096:(q + 1) * 4096], writes=[("aall", q)])
            bisect(aall[:, :], SEQ, 2 * SEQ // NE, [("aall", q) for q in range(4)], 2 * b)
            if need_ctx:
                kb.dma("sp", actx[:], affC[b, :, :], writes=["actx"])
                bisect(actx[:, :], NCTX, 2 * NCTX // NE, ["actx"], 2 * b + 1)
        segs = []
        for b in range(2):
            segs.append((b * NBT, b * NBT + SEQ, 2 * b))
            if need_ctx:
                segs.append((b * NBT + SEQ, (b + 1) * NBT, 2 * b + 1))
        for (s0, s1, col) in segs:
            for c0 in range(s0, s1, 4096):
                n = min(4096, s1 - c0)
                kb.dma("sp", aown[:, 0:n], affS[:, c0:c0 + n], writes=["aown"])
                kb.op("dve", lambda e: e.tensor_scalar(out=msk[:, 0:n], in0=aown[:, 0:n], scalar1=thr[:, col:col + 1], scalar2=None, op0=ALU.is_gt),
                      reads=["aown", ("thr", col)], writes=["msk"])
                kb.op("dve", lambda e: e.tensor_tensor(out=gs_t[:, 0:n], in0=msk[:, 0:n], in1=aown[:, 0:n], op=ALU.mult),
                      reads=["msk", "aown"], writes=["gs_t"])
                kb.dma("sp", gsel_d[:, c0:c0 + n], gs_t[:, 0:n], reads=["gs_t"], writes=["gsel_d"])
        cx.barrier()

    MAXT = PART_T * 128
    oh = cx.sb("oh", [NE, NE, 128])
    kb.op("pool", lambda e: e.memset(oh[:], 1.0), writes=["oh"])
    kb.op("pool", lambda e: e.affine_select(out=oh[:], in_=oh[:], pattern=[[-1, NE], [0, 128]], compare_op=ALU.is_equal, fill=0.0,
                                            base=0, channel_multiplier=1), reads=["oh"], writes=["oh"])
    gsel = cx.sb("gsel", [NE, MAXT])
    hsb = cx.sb("hsb", [128, 8, MAXT], BF16)
    yacc = cx.sb("yacc", [128, 8, MAXT])
    gbc = cx.sb("gbc", [128, MAXT])
    stg = cx.sb("stg", [128, 8, 256])
    stu = cx.sb("stu", [128, 8, 256])
    std = cx.sb("std", [128, 2, D])
    wgb = [cx.sb(f"wgb{i}", [128, 8, 256], BF16) for i in range(2)]
    wub = [cx.sb(f"wub{i}", [128, 8, 256], BF16) for i in range(2)]
    wdb = [cx.sb(f"wdb{i}", [128, 2, D], BF16) for i in range(2)]
    sg = cx.sb("sg", [128, 2, 512])
    ug = cx.sb("ug", [128, 2, 512])
    act = [cx.sb(f"act{i}", [128, 2, 512], BF16) for i in range(2)]
    pb = [cx.sb(f"pb{i}", [128, D], BF16) for i in range(2)]
    gps = cx.ps("gps", [128, 2, 512])
    ups = cx.ps("ups", [128, 2, 512])
    yps = [cx.ps(f"yps{i}", [128, 512]) for i in range(3)]
    pgb = cx.ps("pgb", [128, 512])
    h2T_v = h2T.rearrange("(c p) t -> p c t", p=128)
    cnt = {"w": 0, "y": 0, "a": 0}

    ntl_all = NALL // 128
    for t0 in range(0, ntl_all, PART_T):
        ntl = min(PART_T, ntl_all - t0)
        tok0, ntok = t0 * 128, ntl * 128
        blocks = [(b, min(512, ntok - b)) for b in range(0, ntok, 512)]
        kb.dma("sp", hsb[:, :, 0:ntok], h2T_v[:, :, tok0:tok0 + ntok], writes=["hsb"], slot=("hsb",))
        kb.dma("pool", gsel[:, 0:ntok], gsel_d[:, tok0:tok0 + ntok], reads=["gsel_d"], writes=["gsel"], slot=("gsel",))
        kb.op("pool", lambda e: e.memset(yacc[:], 0.0), writes=[("yacc", dc) for dc in range(8)])
        for ex in range(NEC):
            for (b0, n) in blocks:
                kb.op("pe", lambda e: e.matmul(pgb[:, 0:n], lhsT=oh[:, ex, :], rhs=gsel[:, b0:b0 + n], start=True, stop=True),
                      reads=["oh", "gsel"], writes=["pgb"])
                kb.op("act", lambda e: e.copy(out=gbc[:, b0:b0 + n], in_=pgb[:, 0:n]), reads=["pgb"], writes=["gbc"])
            for jp in range(DFF // 256):
                ws = cnt["w"] % 2
                cnt["w"] += 1
                f0 = jp * 256
                kb.dma("sp", stg[:], wg[ex].rearrange("(c p) f -> p c f", p=128)[:, :, f0:f0 + 256], writes=["stg"], slot=("stg",))
                kb.dma("sp", stu[:], wu[ex].rearrange("(c p) f -> p c f", p=128)[:, :, f0:f0 + 256], writes=["stu"], slot=("stu",))
                kb.dma("pool", std[:], wd[ex, f0:f0 + 256, :].rearrange("(j p) d -> p j d", p=128), writes=["std"], slot=("std",))
                kb.op("pool", lambda e: e.tensor_copy(out=wgb[ws][:], in_=stg[:]), reads=["stg"], writes=[("wgb", ws)])
                kb.op("pool", lambda e: e.tensor_copy(out=wub[ws][:], in_=stu[:]), reads=["stu"], writes=[("wub", ws)])
                kb.op("pool", lambda e: e.tensor_copy(out=wdb[ws][:], in_=std[:]), reads=["std"], writes=[("wdb", ws)])
                for (b0, n) in blocks:
                    ai = cnt["a"] % 2
                    cnt["a"] += 1
                    for jj in range(2):
                        for c in range(8):
                            kb.op("pe", lambda e: e.matmul(gps[:, jj, 0:n], lhsT=wgb[ws][:, c, jj * 128:(jj + 1) * 128], rhs=hsb[:, c, b0:b0 + n],
                                                           start=(c == 0), stop=(c == 7)),
                                  reads=[("wgb", ws), "hsb"], writes=[("gps", jj)])
                        for c in range(8):
                            kb.op("pe", lambda e: e.matmul(ups[:, jj, 0:n], lhsT=wub[ws][:, c, jj * 128:(jj + 1) * 128], rhs=hsb[:, c, b0:b0 + n],
                                                           start=(c == 0), stop=(c == 7)),
                                  reads=[("wub", ws), "hsb"], writes=[("ups", jj)])
                    for jj in range(2):
                        kb.op("act", lambda e: e.activation(out=sg[:, jj, 0:n], in_=gps[:, jj, 0:n], func=AF.Silu), reads=[("gps", jj)], writes=[("sg", jj)])
                        kb.op("dve", lambda e: e.tensor_tensor(out=ug[:, jj, 0:n], in0=ups[:, jj, 0:n], in1=gbc[:, b0:b0 + n], op=ALU.mult),
                              reads=[("ups", jj), "gbc"], writes=[("ug", jj)])
                        kb.op("pool", lambda e: e.tensor_tensor(out=act[ai][:, jj, 0:n], in0=sg[:, jj, 0:n], in1=ug[:, jj, 0:n], op=ALU.mult),
                              reads=[("sg", jj), ("ug", jj)], writes=[("act", ai, jj)])
                    for dc in range(8):
                        yi = cnt["y"] % 3
                        cnt["y"] += 1
                        for jj in range(2):
                            kb.op("pe", lambda e: e.matmul(yps[yi][:, 0:n], lhsT=wdb[ws][:, jj, dc * 128:(dc + 1) * 128], rhs=act[ai][:, jj, 0:n],
                                                           start=(jj == 0), stop=(jj == 1)),
                                  reads=[("wdb", ws), ("act", ai, jj)], writes=[("yps", yi)])
                        kb.op("dve", lambda e: e.tensor_tensor(out=yacc[:, dc, b0:b0 + n], in0=yps[yi][:, 0:n], in1=yacc[:, dc, b0:b0 + n], op=ALU.add),
                              reads=[("yps", yi), ("yacc", dc)], writes=[("yacc", dc)])
        for tt in range(ntl):
            t = t0 + tt
            s = tt % 2
            for dc in range(8):
                kb.op("pe", lambda e: e.transpose(out=gps[:, dc // 4, (dc % 4) * 128:(dc % 4 + 1) * 128], in_=yacc[:, dc, tt * 128:(tt + 1) * 128],
                                                  identity=identf[:]),
                      reads=[("yacc", dc), "ident_f"], writes=[("gps", dc // 4)])
            for hb_ in range(2):
                kb.op("act", lambda e: e.copy(out=pb[s][:, hb_ * 512:(hb_ + 1) * 512], in_=gps[:, hb_, :]), reads=[("gps", hb_)], writes=[("pb", s, hb_)])
            kb.dma("sp", part_o[t * 128:(t + 1) * 128, :], pb[s][:], reads=[("pb", s, 0), ("pb", s, 1)], writes=["part_d"], slot=("pbo", s))
    return cx.finish()


def build_C(l):
    need_ctx = (l == 0)
    NQ = NTOK if need_ctx else NT
    cx = Ctx()
    nc, kb = cx.nc, cx.kb
    x1 = cx.din("x1", [NQ, D])
    parts = cx.din("parts", [NE // NEC, NQ, D], BF16)
    modv = cx.din("modv", [2, 6 * D])
    x2_o = cx.dout("x2", [NQ, D])
    G2 = cx.sb("G2", [128, 2, D])
    for m in range(2):
        kb.dma("sp", G2[:, m, :], modv[m:m + 1, 5 * D:6 * D].broadcast_to([128, D]), writes=[("G2", m)])
    xt = [cx.sb(f"xt{i}", [128, D]) for i in range(2)]
    pt = [cx.sb(f"pt{i}", [128, NE // NEC, D], BF16) for i in range(2)]
    acc = cx.sb("acc", [128, D])
    ot = [cx.sb(f"ot{i}", [128, D]) for i in range(2)]
    for t in range(NQ // 128):
        m = 0 if t < NT // 128 else 1
        s = t % 2
        dq = "sp" if s == 0 else "pool"
        kb.dma(dq, xt[s][:], x1[t * 128:(t + 1) * 128, :], writes=[("xt", s)], slot=("xtC", s))
        kb.dma(dq, pt[s][:], parts[:, t * 128:(t + 1) * 128, :].rearrange("k p d -> p k d"), writes=[("pt", s)], slot=("ptC", s))
        kb.op("dve", lambda e: e.tensor_tensor(out=acc[:], in0=pt[s][:, 0, :], in1=pt[s][:, 1, :], op=ALU.add), reads=[("pt", s)], writes=["acc"])
        for k in range(2, NE // NEC):
            kb.op("dve", lambda e: e.tensor_tensor(out=acc[:], in0=acc[:], in1=pt[s][:, k, :], op=ALU.add), reads=["acc", ("pt", s)], writes=["acc"])
        kb.op("dve", lambda e: e.tensor_tensor(out=acc[:], in0=acc[:], in1=G2[:, m, :], op=ALU.mult), reads=["acc", ("G2", m)], writes=["acc"])
        kb.op("pool", lambda e: e.tensor_tensor(out=ot[s][:], in0=acc[:], in1=xt[s][:], op=ALU.add), reads=["acc", ("xt", s)], writes=[("ot", s)])
        kb.dma(dq, x2_o[t * 128:(t + 1) * 128, :], ot[s][:], reads=[("ot", s)], writes=["x2_d"], slot=("x2C", s))
    return cx.finish()


def run_E2(inp, l, resA):
    nc = _get_nc("E2", build_E2, l)
    need_ctx = (l == 0)
    affs = []
    h2s = []
    for b in range(2):
        grp = [resA[4 * b + rr] for rr in range(4)]
        a = [g["aff"][0:NT] for g in grp]
        h = [g["h2T"][:, 0:NT] for g in grp]
        if need_ctx:
            a.append(grp[0]["aff"][NT:NTOK])
            h.append(grp[0]["h2T"][:, NT:NTOK])
        affs.append(np.concatenate(a, axis=0))
        h2s.append(np.concatenate(h, axis=1))
    h2T_all = np.ascontiguousarray(np.concatenate(h2s, axis=1))
    affT_all = np.concatenate(affs, axis=0).T
    in_maps = []
    for k in range(NE // NEC):
        perm = list(range(NEC * k, NEC * (k + 1))) + [e for e in range(NE) if not (NEC * k <= e < NEC * (k + 1))]
        affM = np.stack([affs[b][0:SEQ].T[perm] for b in range(2)], axis=0)
        if need_ctx:
            affC = np.stack([affs[b][SEQ:SEQ + NCTX].T[perm] for b in range(2)], axis=0)
        else:
            affC = np.zeros((2, NE, NCTX), np.float32)
        in_maps.append({
            "h2T": h2T_all, "affM": np.ascontiguousarray(affM), "affC": np.ascontiguousarray(affC),
            "affS": np.ascontiguousarray(affT_all[perm]),
            "wg": np.ascontiguousarray(inp["w_gate"][l][NEC * k:NEC * (k + 1)]),
            "wu": np.ascontiguousarray(inp["w_up"][l][NEC * k:NEC * (k + 1)]),
            "wd": np.ascontiguousarray(inp["w_down"][l][NEC * k:NEC * (k + 1)]),
        })
    res = run_bass_kernel_spmd(nc, in_maps, core_ids=list(range(NE // NEC)))
    return res.results


def run_C(l, resA, resE, modvs):
    nc = _get_nc("C", build_C, l)
    need_ctx = (l == 0)
    NBT = SEQ + (NCTX if need_ctx else 0)
    in_maps = []
    for c in range(NCORE):
        b, r = c // 4, c % 4
        ps = []
        for k in range(NE // NEC):
            p = resE[k]["part"]
            rows = [p[b * NBT + r * NT:b * NBT + (r + 1) * NT]]
            if need_ctx:
                rows.append(p[b * NBT + SEQ:b * NBT + SEQ + NCTX])
            ps.append(np.concatenate(rows, axis=0))
        in_maps.append({"x1": resA[c]["x1"], "parts": np.ascontiguousarray(np.stack(ps, axis=0)), "modv": modvs[c]})
    res = run_bass_kernel_spmd(nc, in_maps, core_ids=list(range(NCORE)))
    return res.results


def kernel(**inp):
    inp = {k: np.asarray(v) for k, v in inp.items()}
    x_cur = np.asarray(inp["x"], np.float32)
    xc_cur = np.asarray(inp["ctx"], np.float32)
    rope_tab = _rope_tables()
    for l in range(2):
        resP = run_P(inp, l, x_cur, xc_cur, rope_tab)
        modvs = [resP[c]["modv"] for c in range(NCORE)]
        resA, _ = run_A(inp, l, resP, x_cur, xc_cur)
        del resP
        resE = run_E2(inp, l, resA)
        resC = run_C(l, resA, resE, modvs)
        del resA, resE
        x_cur = np.stack([np.concatenate([resC[4 * b + r]["x2"][0:NT] for r in range(4)], axis=0) for b in range(2)], axis=0)
        if l == 0:
            xc_cur = np.stack([resC[4 * b]["x2"][NT:NTOK] for b in range(2)], axis=0)
    return np.ascontiguousarray(x_cur.astype(np.float32))
```

```python
import contextlib
import math
import numpy as np
import ml_dtypes
import concourse.bass as bass
import concourse.mybir as mybir
from concourse.bass_utils import run_bass_kernel_spmd

F32 = mybir.dt.float32
BF16 = mybir.dt.bfloat16
AF = mybir.ActivationFunctionType
ALU = mybir.AluOpType
AX = mybir.AxisListType
NPBF = ml_dtypes.bfloat16

D = 1024
SEQ = 16384
NCORE = 8
NT = 4096
NCTX = 256
NTOK = NT + NCTX
NTL = NTOK // 128
NKEY = SEQ + NCTX
NKT = NKEY // 128
DIN = 2464
DFF = 2816
NE = 16
EPS = 1e-6
QF_W = 1152
KF_W = 1024
NVH = 14
VF_W = NVH * 65
NA_HALO = 3
NA_T = 2 + 32 + 2 * NA_HALO


SEM_ROTATE = 30000


class KB:
    def __init__(self, nc, stack):
        self.nc = nc
        self.stack = stack
        self.eng = {"pe": nc.tensor, "act": nc.scalar, "dve": nc.vector,
                    "pool": nc.gpsimd, "sp": nc.sync}
        self.nsem = 0
        self.cur = {}
        self.pe_sems = set()
        for e in self.eng:
            self.cur[e] = [self._newsem(e), 0]
            if e == "pe":
                self.pe_sems.add(id(self.cur[e][0]))
        self.waited = {}
        self.last_w = {}
        self.readers = {}
        self.dma_sems = {}
        self.n_instr = 0

    def _newsem(self, tag):
        self.nsem += 1
        return self.stack.enter_context(self.nc.semaphore(f"s{self.nsem}_{tag}"))

    def _wait(self, e, sem, val):
        if e == "pe" and id(sem) in self.pe_sems:
            return
        key = (e, id(sem))
        if self.waited.get(key, 0) >= val:
            return
        self.waited[key] = val
        self.eng[e].wait_ge(sem, val)
        self.n_instr += 1

    def _deps(self, e, reads, writes):
        need = {}

        def add(rec):
            if rec is None:
                return
            sem, val = rec
            k = id(sem)
            if k not in need or need[k][1] < val:
                need[k] = (sem, val)
        for r in reads:
            add(self.last_w.get(r))
        for w in writes:
            add(self.last_w.get(w))
            for rec in self.readers.get(w, ()):
                add(rec)
        for sem, val in need.values():
            self._wait(e, sem, val)

    def _commit(self, rec, reads, writes):
        for r in reads:
            lst = self.readers.setdefault(r, [])
            lst.append(rec)
            if len(lst) > 8:
                best = {}
                for s, v in lst:
                    if id(s) not in best or best[id(s)][1] < v:
                        best[id(s)] = (s, v)
                self.readers[r] = list(best.values())
        for w in writes:
            self.last_w[w] = rec
            self.readers[w] = []

    def op(self, e, fn, reads=(), writes=()):
        self._deps(e, reads, writes)
        ins = fn(self.eng[e])
        st = self.cur[e]
        if st[1] >= SEM_ROTATE:
            st[0] = self._newsem(e)
            st[1] = 0
            if e == "pe":
                self.pe_sems.add(id(st[0]))
        st[1] += 1
        ins.then_inc(st[0], 1)
        self.n_instr += 1
        self._commit((st[0], st[1]), reads, writes)
        return ins

    def dma(self, q, out, in_, reads=(), writes=(), slot=None, **kw):
        self._deps(q, reads, writes)
        if slot is None:
            slot = ("auto",) + tuple(writes) + tuple(reads)
        st = self.dma_sems.get(slot)
        if st is None or st[1] >= SEM_ROTATE:
            st = [self._newsem("d"), 0]
            self.dma_sems[slot] = st
        ins = self.eng[q].dma_start(out=out, in_=in_, **kw)
        st[1] += 16
        ins.then_inc(st[0], 16)
        self.n_instr += 1
        self._commit((st[0], st[1]), reads, writes)
        return ins

    def wait_all(self, e):
        recs = {}
        for rec in list(self.last_w.values()) + [x for l in self.readers.values() for x in l]:
            s, v = rec
            if id(s) not in recs or recs[id(s)][1] < v:
                recs[id(s)] = (s, v)
        for s, v in recs.values():
            self._wait(e, s, v)


class Ctx:
    def __init__(self):
        self.nc = bass.Bass("TRN2", target_bir_lowering=False)
        self.stack = contextlib.ExitStack()
        self.kb = KB(self.nc, self.stack)
        self._n = 0

    def din(self, name, shape, dt=F32):
        return self.nc.dram_tensor(name, list(shape), dt, kind="ExternalInput").ap()

    def dout(self, name, shape, dt=F32):
        return self.nc.dram_tensor(name, list(shape), dt, kind="ExternalOutput").ap()

    def dint(self, name, shape, dt=F32):
        return self.nc.dram_tensor(name, list(shape), dt, kind="Internal").ap()

    def sb(self, name, shape, dt=F32, stack=None):
        return (stack or self.stack).enter_context(self.nc.sbuf_tensor(name, list(shape), dt))

    def ps(self, name, shape, dt=F32, stack=None):
        return (stack or self.stack).enter_context(self.nc.psum_tensor(name, list(shape), dt))

    def barrier(self):
        for e in self.kb.eng:
            self.kb.wait_all(e)

    def finish(self):
        self.kb.wait_all("sp")
        self.stack.close()
        return self.nc


def make_ident(cx, name="ident"):
    kb = cx.kb
    identf = cx.sb(name + "_f", [128, 128], F32)
    identb = cx.sb(name + "_b", [128, 128], BF16)
    kb.op("pool", lambda e: e.memset(identf[:], 1.0), writes=[name + "_f"])
    kb.op("pool", lambda e: e.affine_select(out=identf[:], in_=identf[:], pattern=[[-1, 128]],
                                            compare_op=ALU.is_equal, fill=0.0, base=0, channel_multiplier=1),
          reads=[name + "_f"], writes=[name + "_f"])
    kb.op("dve", lambda e: e.tensor_copy(out=identb[:], in_=identf[:]), reads=[name + "_f"], writes=[name + "_b"])
    return identf, identb


def rstd_from_ss(cx, ss_ap, out_ap, width, eps_ap, rkeys, wkey):
    kb = cx.kb
    kb.op("act", lambda e: e.activation(out=out_ap, in_=ss_ap, func=AF.Sqrt, scale=1.0 / width, bias=eps_ap),
          reads=list(rkeys) + ["eps"], writes=[wkey])
    kb.op("dve", lambda e: e.reciprocal(out=out_ap, in_=out_ap), reads=[wkey], writes=[wkey])


def build_P():
    cx = Ctx()
    nc, kb = cx.nc, cx.kb
    xin = cx.din("xin", [NTOK, D])
    cs2 = cx.din("cs2", [2, D])
    wmod = cx.din("wmod", [D, 6 * D])
    bmod = cx.din("bmod", [1, 6 * D])
    gattn = cx.din("gattn", [1, D])
    win = cx.din("win", [D, DIN])
    gains = cx.din("gains", [1, DIN])
    wuq = cx.din("wuq", [256, 384])
    wukv = cx.din("wukv", [128, 512])
    rope = cx.din("rope", [NT, 192])
    modv = cx.dout("modv", [2, 6 * D])
    qT_o = cx.dout("qT", [QF_W, NTOK], BF16)
    kT_o = cx.dout("kT", [KF_W, NTOK], BF16)
    v_o = cx.dout("v", [NTL, 128, VF_W], BF16)

    identf, identb = make_ident(cx)
    eps = cx.sb("eps", [128, 1])
    kb.op("dve", lambda e: e.memset(eps[:], EPS), writes=["eps"])

    Abc = cx.sb("Abc", [128, 2, D])
    Bbc = cx.sb("Bbc", [128, 2, D])
    with contextlib.ExitStack() as ph:
        cs_sb = cx.sb("cs_sb", [2, D], stack=ph)
        bm_sb = cx.sb("bm_sb", [2, 6 * D], stack=ph)
        modrow = cx.sb("modrow", [2, 6 * D], stack=ph)
        scT = cx.sb("scT", [128, 8, 2], stack=ph)
        wm = [cx.sb(f"wm{i}", [128, 8, 512], stack=ph) for i in range(2)]
        gb = cx.sb("gb", [128, D], stack=ph)
        tmpb = cx.sb("tmpb", [128, D], stack=ph)
        ps_t = cx.ps("ps_mt", [128, 512], stack=ph)
        ps_m = [cx.ps(f"ps_mm{i}", [128, 512], stack=ph) for i in range(2)]
        kb.dma("sp", cs_sb[:], cs2[:, :], writes=["cs_sb"])
        kb.dma("sp", bm_sb[:], bmod[0:1, :].broadcast_to([2, 6 * D]), writes=["bm_sb"])
        kb.op("act", lambda e: e.activation(out=cs_sb[:], in_=cs_sb[:], func=AF.Silu), reads=["cs_sb"], writes=["cs_sb"])
        for c in range(8):
            kb.op("pe", lambda e: e.transpose(out=ps_t[:, c * 2:c * 2 + 2], in_=cs_sb[0:2, c * 128:(c + 1) * 128],
                                              identity=identf[0:2, 0:2]),
                  reads=["cs_sb", "ident_f"], writes=[("ps_mt", c)])
        kb.op("dve", lambda e: e.tensor_copy(out=scT[:].rearrange("p c r -> p (c r)"), in_=ps_t[:, 0:16]),
              reads=[("ps_mt", c) for c in range(8)], writes=["scT"])
        wm_v = wmod.rearrange("(c p) n -> p c n", p=128)
        for cb in range(12):
            s = cb % 2
            kb.dma("sp" if s == 0 else "pool", wm[s][:], wm_v[:, :, cb * 512:(cb + 1) * 512], writes=[("wm", s)], slot=("wm", s))
            for c in range(8):
                kb.op("pe", lambda e: e.matmul(ps_m[s][0:2, :], lhsT=scT[:, c, :], rhs=wm[s][:, c, :],
                                               start=(c == 0), stop=(c == 7)),
                      reads=[("wm", s), "scT"], writes=[("ps_mm", s)])
            kb.op("dve", lambda e: e.tensor_tensor(out=modrow[:, cb * 512:(cb + 1) * 512], in0=ps_m[s][0:2, :],
                                                   in1=bm_sb[:, cb * 512:(cb + 1) * 512], op=ALU.add),
                  reads=[("ps_mm", s), "bm_sb"], writes=["modrow"])
        kb.dma("sp", modv[:, :], modrow[:], reads=["modrow"], writes=["modv_d"])
        kb.dma("sp", gb[:], gattn[0:1, :].broadcast_to([128, D]), writes=["gb"])
        for m in range(2):
            kb.dma("sp", tmpb[:], modv[m:m + 1, D:2 * D].broadcast_to([128, D]), reads=["modv_d"], writes=["tmpb"])
            kb.op("dve", lambda e: e.scalar_tensor_tensor(out=Abc[:, m, :], in0=tmpb[:], scalar=1.0, in1=gb[:],
                                                          op0=ALU.add, op1=ALU.mult),
                  reads=["tmpb", "gb"], writes=[("Abc", m)])
            kb.dma("pool", Bbc[:, m, :], modv[m:m + 1, 0:D].broadcast_to([128, D]), reads=["modv_d"], writes=[("Bbc", m)])
        cx.barrier()

    win_bf = cx.sb("win_bf", [128, 8, DIN], BF16)
    wuq_bf = cx.sb("wuq_bf", [128, 2, 384], BF16)
    wukv_bf = cx.sb("wukv_bf", [128, 512], BF16)
    gains_sb = cx.sb("gains_sb", [128, DIN])
    kb.dma("pool", gains_sb[:], gains[0:1, :].broadcast_to([128, DIN]), writes=["gains"])
    with contextlib.ExitStack() as ph:
        wst = [cx.sb(f"wst{i}", [128, DIN], stack=ph) for i in range(2)]
        for c in range(8):
            s = c % 2
            kb.dma("sp" if s == 0 else "pool", wst[s][:], win[c * 128:(c + 1) * 128, :], writes=[("wst", s)], slot=("wst", s))
            kb.op("dve" if s == 0 else "pool", lambda e: e.tensor_copy(out=win_bf[:, c, :], in_=wst[s][:]),
                  reads=[("wst", s)], writes=["win_bf"])
        for c in range(2):
            kb.dma("sp", wst[0][:, 0:384], wuq[c * 128:(c + 1) * 128, :], writes=[("wst", 0)], slot=("wst", 0))
            kb.op("dve", lambda e: e.tensor_copy(out=wuq_bf[:, c, :], in_=wst[0][:, 0:384]), reads=[("wst", 0)], writes=["wuq_bf"])
        kb.dma("sp", wst[1][:, 0:512], wukv[:, :], writes=[("wst", 1)], slot=("wst", 1))
        kb.op("dve", lambda e: e.tensor_copy(out=wukv_bf[:], in_=wst[1][:, 0:512]), reads=[("wst", 1)], writes=["wukv_bf"])
        cx.barrier()

    xt = [cx.sb(f"xt{i}", [128, D]) for i in range(2)]
    rp = [cx.sb(f"rp{i}", [128, 192]) for i in range(2)]
    junk = cx.sb("junk", [128, D])
    st_ss = cx.sb("st_ss", [128, 1])
    st_rstd = cx.sb("st_rstd", [128, 1])
    tmp = cx.sb("tmp", [128, D])
    hb = cx.sb("hb", [128, D], BF16)
    hT = cx.sb("hT", [128, D], BF16)
    pj = cx.sb("pj", [128, DIN])
    sq = cx.sb("sq", [128, 544])
    nss = cx.sb("nss", [128, 32])
    nrs = cx.sb("nrs", [128, 32])
    ntmp = cx.sb("ntmp", [128, 544])
    n32 = cx.sb("n32", [128, 544])
    n64 = cx.sb("n64", [128, 384])
    cqn = cx.sb("cqn", [128, 256], BF16)
    ckvn = cx.sb("ckvn", [128, 128], BF16)
    cT = cx.sb("cT", [128, 384], BF16)
    mq = cx.sb("mq", [128, 384])
    mqn = cx.sb("mqn", [128, 384])
    mkv = cx.sb("mkv", [128, 512])
    r1 = cx.sb("r1", [128, 544])
    r2 = cx.sb("r2", [128, 544])
    QF = [cx.sb(f"QF{i}", [128, QF_W], BF16) for i in range(2)]
    KF = [cx.sb(f"KF{i}", [128, KF_W], BF16) for i in range(2)]
    VF = [cx.sb(f"VF{i}", [128, NVH, 65], BF16) for i in range(2)]
    sQ = [cx.sb(f"sQ{i}", [128, 9, 512], BF16) for i in range(2)]
    sK = [cx.sb(f"sK{i}", [128, 8, 512], BF16) for i in range(2)]
    ps_tr = cx.ps("ps_tr", [128, D], BF16)
    ps_pj = [cx.ps(f"ps_pj{i}", [128, 512]) for i in range(2)]
    ps_c = cx.ps("ps_c", [128, 1024], BF16)
    ps_up = cx.ps("ps_up", [128, 512])
    ps_q = [cx.ps(f"ps_q{i}", [128, 1024], BF16) for i in range(3)]

    for i in range(2):
        kb.op("pool", lambda e: e.memset(KF[i][:], 0.0), writes=[("KF", i)])
        kb.op("pool", lambda e: e.memset(VF[i][:], 1.0), writes=[("VF", i)])

    GO = {"naqk": 0, "w32": 512, "gqa": 1056, "nope": 1440, "cq": 1696, "ckv": 1952, "mq": 2080}

    def gnorm(src3, G, w, rkeys, scratch_w):
        sq3 = sq[:, 0:G * w].rearrange("p (g w) -> p g w", w=w)
        kb.op("dve", lambda e: e.tensor_tensor(out=sq3, in0=src3, in1=src3, op=ALU.mult), reads=rkeys, writes=["sq"])
        kb.op("dve", lambda e: e.tensor_reduce(out=nss[:, 0:G], in_=sq3, axis=AX.X, op=ALU.add), reads=["sq"], writes=["nss"])
        rstd_from_ss(cx, nss[:, 0:G], nrs[:, 0:G], w, eps[:, 0:1], ["nss"], "nrs")
        o3 = ntmp[:, 0:G * w].rearrange("p (g w) -> p g w", w=w)
        kb.op("dve", lambda e: e.tensor_tensor(out=o3, in0=src3, in1=nrs[:, 0:G].unsqueeze(2).broadcast_to([128, G, w]),
                                               op=ALU.mult), reads=list(rkeys) + ["nrs"], writes=["ntmp"])
        return o3

    def gain3(off, G, w):
        return gains_sb[:, off:off + G * w].rearrange("p (g w) -> p g w", w=w)

    def rope_apply(src3, G, n, tab, toff, dsts, rkeys, rpk):
        C = tab[:, toff:toff + n].unsqueeze(1).broadcast_to([128, G, n])
        q4 = n // 4
        t1 = r1[:, 0:G * n].rearrange("p (g w) -> p g w", w=n)
        t2 = r2[:, 0:G * n].rearrange("p (g w) -> p g w", w=n)
        kb.op("dve", lambda e: e.tensor_tensor(out=t1, in0=src3, in1=C, op=ALU.mult), reads=list(rkeys) + [rpk], writes=["r1"])
        s5 = src3.rearrange("p g (r x w) -> p g r x w", r=2, x=2, w=q4)
        t5 = t2.rearrange("p g (r x w) -> p g r x w", r=2, x=2, w=q4)
        S5 = tab[:, toff + n:toff + 2 * n].rearrange("p (r x w) -> p r x w", r=2, x=2, w=q4)
        for xh in range(2):
            Sx = S5[:, :, xh, :].unsqueeze(1).broadcast_to([128, G, 2, q4])
            kb.op("dve", lambda e: e.tensor_tensor(out=t5[:, :, :, xh, :], in0=s5[:, :, :, 1 - xh, :], in1=Sx, op=ALU.mult),
                  reads=list(rkeys) + [rpk], writes=[("r2", xh)])
        for (g0, g1, dst3, wkey) in dsts:
            kb.op("dve", lambda e: e.tensor_tensor(out=dst3, in0=t1[:, g0:g1, :], in1=t2[:, g0:g1, :], op=ALU.add),
                  reads=["r1", ("r2", 0), ("r2", 1)], writes=[wkey])

    rope_v = rope
    qT_v = qT_o.rearrange("(k p) t -> p k t", p=128)
    kT_v = kT_o.rearrange("(k p) t -> p k t", p=128)

    for t in range(NTL):
        m = 0 if t < NT // 128 else 1
        s = t % 2
        grp = t // 4
        gs = grp % 2
        tin = t % 4
        dq = "sp" if s == 0 else "pool"
        kb.dma(dq, xt[s][:], xin[t * 128:(t + 1) * 128, :], writes=[("xt", s)], slot=("xt", s))
        if m == 0:
            kb.dma(dq, rp[s][:], rope_v[t * 128:(t + 1) * 128, :], writes=[("rp", s)], slot=("rp", s))
        kb.op("act", lambda e: e.activation(out=junk[:], in_=xt[s][:], func=AF.Square, accum_out=st_ss[:]),
              reads=[("xt", s)], writes=["junk", "st_ss"])
        rstd_from_ss(cx, st_ss[:], st_rstd[:], D, eps[:, 0:1], ["st_ss"], "st_rstd")
        kb.op("dve", lambda e: e.scalar_tensor_tensor(out=tmp[:], in0=xt[s][:], scalar=st_rstd[:, 0:1], in1=Abc[:, m, :],
                                                      op0=ALU.mult, op1=ALU.mult),
              reads=[("xt", s), "st_rstd", ("Abc", m)], writes=["tmp"])
        kb.op("pool", lambda e: e.tensor_tensor(out=hb[:], in0=tmp[:], in1=Bbc[:, m, :], op=ALU.add),
              reads=["tmp", ("Bbc", m)], writes=["hb"])
        for c in range(8):
            kb.op("pe", lambda e: e.transpose(out=ps_tr[:, c * 128:(c + 1) * 128], in_=hb[:, c * 128:(c + 1) * 128], identity=identb[:]),
                  reads=["hb", "ident_b"], writes=[("ps_tr", c)])
        kb.op("act", lambda e: e.copy(out=hT[:], in_=ps_tr[:]), reads=[("ps_tr", c) for c in range(8)], writes=["hT"])
        for cb in range(5):
            c0 = cb * 512
            wd = min(512, DIN - c0)
            pp = ps_pj[cb % 2]
            for c in range(8):
                kb.op("pe", lambda e: e.matmul(pp[:, 0:wd], lhsT=hT[:, c * 128:(c + 1) * 128], rhs=win_bf[:, c, c0:c0 + wd],
                                               start=(c == 0), stop=(c == 7)),
                      reads=["hT", "win_bf"], writes=[("ps_pj", cb % 2)])
            kb.op("act", lambda e: e.copy(out=pj[:, c0:c0 + wd], in_=pp[:, 0:wd]), reads=[("ps_pj", cb % 2)], writes=[("pj", cb)])
        PJ = [("pj", cb) for cb in range(5)]
        qf, kf, vf = QF[s], KF[s], VF[s]
        qk, kk, vk = ("QF", s), ("KF", s), ("VF", s)
        o3 = gnorm(pj[:, 0:512].rearrange("p (g w) -> p g w", w=64), 8, 64, PJ, 512)
        kb.op("dve", lambda e: e.tensor_tensor(out=qf[:, 0:256].rearrange("p (g w) -> p g w", w=64), in0=o3[:, 0:4, :],
                                               in1=gain3(GO["naqk"], 8, 64)[:, 0:4, :], op=ALU.mult),
              reads=["ntmp", "gains"], writes=[qk])
        kb.op("dve", lambda e: e.tensor_tensor(out=kf[:, 0:256].rearrange("p (g w) -> p g w", w=64), in0=o3[:, 4:8, :],
                                               in1=gain3(GO["naqk"], 8, 64)[:, 4:8, :], op=ALU.mult),
              reads=["ntmp", "gains"], writes=[kk])
        o3 = gnorm(pj[:, 1152:1696].rearrange("p (g w) -> p g w", w=32), 17, 32, PJ, 544)
        n32_3 = n32[:, 0:544].rearrange("p (g w) -> p g w", w=32)
        d_kr = kf[:, 512:544].rearrange("p (g w) -> p g w", w=32)
        d_dq = qf[:, 640:896].rearrange("p (g w) -> p g w", w=32)
        d_dk = kf[:, 640:896].rearrange("p (g w) -> p g w", w=32)
        if m == 0:
            kb.op("dve", lambda e: e.tensor_tensor(out=n32_3, in0=o3, in1=gain3(GO["w32"], 17, 32), op=ALU.mult),
                  reads=["ntmp", "gains"], writes=["n32"])
            rope_apply(n32_3, 17, 32, rp[s], 0, [(0, 1, d_kr, kk), (1, 9, d_dq, qk), (9, 17, d_dk, kk)], ["n32"], ("rp", s))
        else:
            g3 = gain3(GO["w32"], 17, 32)
            for (g0, g1, dst, wk) in [(0, 1, d_kr, kk), (1, 9, d_dq, qk), (9, 17, d_dk, kk)]:
                kb.op("dve", lambda e: e.tensor_tensor(out=dst, in0=o3[:, g0:g1, :], in1=g3[:, g0:g1, :], op=ALU.mult),
                      reads=["ntmp", "gains"], writes=[wk])
        o3 = gnorm(pj[:, 1952:2336].rearrange("p (g w) -> p g w", w=64), 6, 64, PJ, 384)
        n64_3 = n64[:, 0:384].rearrange("p (g w) -> p g w", w=64)
        d_gq = qf[:, 896:1152].rearrange("p (g w) -> p g w", w=64)
        d_gk = kf[:, 896:1024].rearrange("p (g w) -> p g w", w=64)
        if m == 0:
            kb.op("dve", lambda e: e.tensor_tensor(out=n64_3, in0=o3, in1=gain3(GO["gqa"], 6, 64), op=ALU.mult),
                  reads=["ntmp", "gains"], writes=["n64"])
            rope_apply(n64_3, 6, 64, rp[s], 64, [(0, 4, d_gq, qk), (4, 6, d_gk, kk)], ["n64"], ("rp", s))
        else:
            g3 = gain3(GO["gqa"], 6, 64)
            for (g0, g1, dst, wk) in [(0, 4, d_gq, qk), (4, 6, d_gk, kk)]:
                kb.op("dve", lambda e: e.tensor_tensor(out=dst, in0=o3[:, g0:g1, :], in1=g3[:, g0:g1, :], op=ALU.mult),
                      reads=["ntmp", "gains"], writes=[wk])
        o3 = gnorm(pj[:, 768:1024].rearrange("p (g w) -> p g w", w=256), 1, 256, PJ, 256)
        kb.op("dve", lambda e: e.tensor_tensor(out=cqn[:].rearrange("p (g w) -> p g w", w=256), in0=o3, in1=gain3(GO["cq"], 1, 256), op=ALU.mult),
              reads=["ntmp", "gains"], writes=["cqn"])
        for c in range(2):
            kb.op("pe", lambda e: e.transpose(out=ps_c[:, c * 128:(c + 1) * 128], in_=cqn[:, c * 128:(c + 1) * 128], identity=identb[:]),
                  reads=["cqn", "ident_b"], writes=[("ps_c", c)])
        kb.op("act", lambda e: e.copy(out=cT[:, 0:256], in_=ps_c[:, 0:256]), reads=[("ps_c", 0), ("ps_c", 1)], writes=[("cT", 0)])
        for c in range(2):
            kb.op("pe", lambda e: e.matmul(ps_up[:, 0:384], lhsT=cT[:, c * 128:(c + 1) * 128], rhs=wuq_bf[:, c, :], start=(c == 0), stop=(c == 1)),
                  reads=[("cT", 0), "wuq_bf"], writes=["ps_up"])
        kb.op("act", lambda e: e.copy(out=mq[:], in_=ps_up[:, 0:384]), reads=["ps_up"], writes=["mq"])
        o3 = gnorm(mq[:, 0:384].rearrange("p (g w) -> p g w", w=96), 4, 96, ["mq"], 384)
        mqn3 = mqn[:, 0:384].rearrange("p (g w) -> p g w", w=96)
        d_mq = qf[:, 256:640].rearrange("p (g w) -> p g w", w=96)
        if m == 0:
            kb.op("dve", lambda e: e.tensor_tensor(out=mqn3, in0=o3, in1=gain3(GO["mq"], 4, 96), op=ALU.mult),
                  reads=["ntmp", "gains"], writes=["mqn"])
            kb.op("pool", lambda e: e.tensor_copy(out=d_mq[:, :, 0:64], in_=mqn3[:, :, 0:64]), reads=["mqn"], writes=[qk])
            rope_apply(mqn3[:, :, 64:96], 4, 32, rp[s], 0, [(0, 4, d_mq[:, :, 64:96], qk)], ["mqn"], ("rp", s))
        else:
            kb.op("dve", lambda e: e.tensor_tensor(out=d_mq, in0=o3, in1=gain3(GO["mq"], 4, 96), op=ALU.mult),
                  reads=["ntmp", "gains"], writes=[qk])
        o3 = gnorm(pj[:, 1024:1152].rearrange("p (g w) -> p g w", w=128), 1, 128, PJ, 128)
        kb.op("dve", lambda e: e.tensor_tensor(out=ckvn[:].rearrange("p (g w) -> p g w", w=128), in0=o3, in1=gain3(GO["ckv"], 1, 128), op=ALU.mult),
              reads=["ntmp", "gains"], writes=["ckvn"])
        kb.op("pe", lambda e: e.transpose(out=ps_c[:, 256:384], in_=ckvn[:], identity=identb[:]),
              reads=["ckvn", "ident_b"], writes=[("ps_c", 2)])
        kb.op("act", lambda e: e.copy(out=cT[:, 256:384], in_=ps_c[:, 256:384]), reads=[("ps_c", 2)], writes=[("cT", 1)])
        kb.op("pe", lambda e: e.matmul(ps_up[:, 0:512], lhsT=cT[:, 256:384], rhs=wukv_bf[:], start=True, stop=True),
              reads=[("cT", 1), "wukv_bf"], writes=["ps_up"])
        kb.op("act", lambda e: e.copy(out=mkv[:], in_=ps_up[:, 0:512]), reads=["ps_up"], writes=["mkv"])
        mkv3 = mkv[:, 0:512].rearrange("p (g w) -> p g w", w=128)
        o3 = gnorm(mkv3[:, :, 0:64], 4, 64, ["mkv"], 256)
        kb.op("dve", lambda e: e.tensor_tensor(out=kf[:, 256:512].rearrange("p (g w) -> p g w", w=64), in0=o3,
                                               in1=gain3(GO["nope"], 4, 64), op=ALU.mult),
              reads=["ntmp", "gains"], writes=[kk])
        kb.op("pool", lambda e: e.tensor_copy(out=vf[:, 0:4, 0:64], in_=pj[:, 512:768].rearrange("p (g w) -> p g w", w=64)), reads=PJ, writes=[vk])
        kb.op("pool", lambda e: e.tensor_copy(out=vf[:, 4:8, 0:64], in_=mkv3[:, :, 64:128]), reads=["mkv"], writes=[vk])
        kb.op("pool", lambda e: e.tensor_copy(out=vf[:, 8:12, 0:64], in_=pj[:, 1696:1952].rearrange("p (g w) -> p g w", w=64)), reads=PJ, writes=[vk])
        kb.op("pool", lambda e: e.tensor_copy(out=vf[:, 12:14, 0:64], in_=pj[:, 2336:2464].rearrange("p (g w) -> p g w", w=64)), reads=PJ, writes=[vk])
        kb.dma(dq, v_o[t, :, :], vf[:].rearrange("p g w -> p (g w)"), reads=[vk], writes=["v_d"], slot=("vout", s))
        blocks = [(qf, qk, b, sQ[gs], ("sQ", gs)) for b in range(9)] + [(kf, kk, b, sK[gs], ("sK", gs)) for b in range(8)]
        for bi, (src, skey, b, dst, dkey) in enumerate(blocks):
            pq = ps_q[bi // 8]
            kb.op("pe", lambda e: e.transpose(out=pq[:, (bi % 8) * 128:(bi % 8 + 1) * 128], in_=src[:, b * 128:(b + 1) * 128], identity=identb[:]),
                  reads=[skey, "ident_b"], writes=[("ps_q", bi // 8, bi % 8)])
        kb.op("act", lambda e: e.copy(out=sQ[gs][:, 0:8, tin * 128:(tin + 1) * 128], in_=ps_q[0][:].rearrange("p (b w) -> p b w", w=128)),
              reads=[("ps_q", 0, j) for j in range(8)], writes=[("sQ", gs)])
        kb.op("dve", lambda e: e.tensor_copy(out=sQ[gs][:, 8, tin * 128:(tin + 1) * 128], in_=ps_q[1][:, 0:128]),
              reads=[("ps_q", 1, 0)], writes=[("sQ", gs)])
        kb.op("act", lambda e: e.copy(out=sK[gs][:, 0:7, tin * 128:(tin + 1) * 128], in_=ps_q[1][:, 128:1024].rearrange("p (b w) -> p b w", w=128)),
              reads=[("ps_q", 1, j) for j in range(1, 8)], writes=[("sK", gs)])
        kb.op("dve", lambda e: e.tensor_copy(out=sK[gs][:, 7, tin * 128:(tin + 1) * 128], in_=ps_q[2][:, 0:128]),
              reads=[("ps_q", 2, 0)], writes=[("sK", gs)])
        last_in_grp = (tin == 3) or (t == NTL - 1)
        if last_in_grp:
            ntk = (tin + 1) * 128
            t0 = grp * 512
            kb.dma("sp", qT_v[:, :, t0:t0 + ntk], sQ[gs][:, :, 0:ntk], reads=[("sQ", gs)], writes=["qT_d"], slot=("sQo", gs))
            kb.dma("pool", kT_v[:, :, t0:t0 + ntk], sK[gs][:, :, 0:ntk], reads=[("sK", gs)], writes=["kT_d"], slot=("sKo", gs))
    return cx.finish()


_NC_CACHE = {}


def _get_nc(name, builder, *args):
    key = (name,) + tuple(args)
    if key not in _NC_CACHE:
        _NC_CACHE[key] = builder(*args)
    return _NC_CACHE[key]


def _rope_tables():
    t = np.arange(SEQ)
    row, col = (t // 64).astype(np.float64), (t % 64).astype(np.float64)
    out = np.zeros((SEQ, 192), np.float32)

    def fill(n, off):
        half = n // 2
        inv = 10000.0 ** (-np.arange(0, half, 2, dtype=np.float32).astype(np.float64) / half)
        inv = inv.astype(np.float32)
        for hi, pos in enumerate((row, col)):
            ang = (pos.astype(np.float32)[:, None] * inv[None, :]).astype(np.float32)
            c, s = np.cos(ang), np.sin(ang)
            q4 = n // 4
            b = off + hi * half
            out[:, b:b + q4] = c
            out[:, b + q4:b + 2 * q4] = c
            out[:, off + n + hi * half:off + n + hi * half + q4] = -s
            out[:, off + n + hi * half + q4:off + n + hi * half + 2 * q4] = s
    fill(32, 0)
    fill(64, 64)
    return out


def _gains_vec(inp, l):
    f = lambda k: np.asarray(inp[k][l], np.float32).reshape(-1)
    parts = [np.tile(f("g_na_q"), 4), np.tile(f("g_na_k"), 4),
             f("g_mla_k_rope"), np.tile(f("g_diff_q"), 4), np.tile(f("g_diff_k"), 4),
             np.tile(f("g_gqa_q"), 4), np.tile(f("g_gqa_k"), 2),
             np.tile(f("g_mla_k_nope"), 4), f("g_mla_cq"), f("g_mla_ckv"), np.tile(f("g_mla_q"), 4)]
    v = np.concatenate(parts)[None, :]
    assert v.shape == (1, DIN)
    return np.ascontiguousarray(v)


def run_P(inp, l, x_cur, xc_cur, rope_tab):
    nc = _get_nc("P", build_P)
    in_maps = []
    for c in range(NCORE):
        b, r = c // 4, c % 4
        in_maps.append({
            "xin": np.ascontiguousarray(np.concatenate([x_cur[b, r * NT:(r + 1) * NT], xc_cur[b]], axis=0)),
            "cs2": np.ascontiguousarray(np.stack([inp["c"][b], inp["c_ctx"]], axis=0)),
            "wmod": np.ascontiguousarray(inp["w_mod"][l]),
            "bmod": np.ascontiguousarray(inp["b_mod"][l][None, :]),
            "gattn": np.ascontiguousarray(inp["g_attn"][l][None, :]),
            "win": np.ascontiguousarray(inp["w_in"][l]),
            "gains": _gains_vec(inp, l),
            "wuq": np.ascontiguousarray(inp["w_mla_uq"][l]),
            "wukv": np.ascontiguousarray(inp["w_mla_ukv"][l]),
            "rope": np.ascontiguousarray(rope_tab[r * NT:(r + 1) * NT]),
        })
    res = run_bass_kernel_spmd(nc, in_maps, core_ids=list(range(NCORE)))
    return res.results


def _full_heads():
    hs = []
    for h in range(4):
        hs.append(dict(dk=96, q=[(256 + 96 * h, 96)], k=[(256 + 64 * h, 64), (512, 32)], vh=4 + h,
                       scale=96 ** -0.5, orow=256 + 64 * h, kind="plain"))
    for h in range(4):
        for c in range(2):
            hs.append(dict(dk=32, q=[(640 + 64 * h + 32 * c, 32)], k=[(640 + 64 * h + 32 * c, 32)], vh=8 + h,
                           scale=32 ** -0.5, orow=512 + 64 * h, kind="diff%d" % c))
    for h in range(4):
        hs.append(dict(dk=64, q=[(896 + 64 * h, 64)], k=[(896 + 64 * (h // 2), 64)], vh=12 + h // 2,
                       scale=64 ** -0.5, orow=768 + 64 * h, kind="plain"))
    return hs


def build_A(l, parts="fnp", nheads=16):
    need_ctx = (l == 0)
    lam_init = 0.8 - 0.6 * math.exp(-0.3 * l)
    NQ = NTOK if need_ctx else NT
    NQT = NQ // 128
    cx = Ctx()
    nc, kb = cx.nc, cx.kb
    qT = cx.din("qT", [QF_W, NTOK], BF16)
    kT = cx.din("kT", [KF_W, NKEY], BF16)
    vA = cx.din("v", [NKT, 128, VF_W], BF16)
    kna = cx.din("kna", [256, NA_T * 128], BF16)
    vna = cx.din("vna", [NA_T, 128, 4 * 65], BF16)
    nab = cx.din("nab", [4, 128, 5 * 7 * 128])
    xin = cx.din("xin", [NTOK, D])
    wout = cx.din("wout", [D, D])
    modv = cx.din("modv", [2, 6 * D])
    gffn = cx.din("gffn", [1, D])
    wr = cx.din("wr", [D, NE])
    gsub = cx.din("gsub", [64, 1])
    lamp = cx.din("lamp", [1, 128])
    x1_o = cx.dout("x1", [NQ, D])
    aff_o = cx.dout("aff", [NQ, NE])
    h2T_o = cx.dout("h2T", [D, NQ], BF16)
    oT_d = cx.dint("oT_d", [D, NQ], BF16)

    identf, identb = make_ident(cx)
    eps = cx.sb("eps", [128, 1])
    kb.op("dve", lambda e: e.memset(eps[:], EPS), writes=["eps"])
    sel65 = cx.sb("sel65", [128, 64])
    kb.op("pool", lambda e: e.memset(sel65[:], 0.0), writes=["sel65"])
    kb.op("pool", lambda e: e.memset(sel65[64:65, :], 1.0), reads=["sel65"], writes=["sel65"])
    ones64 = cx.sb("ones64", [64, 64])
    kb.op("pool", lambda e: e.memset(ones64[:], 1.0), writes=["ones64"])
    lam_sb = cx.sb("lam_sb", [64, 128])
    lam_t = cx.sb("lam_t", [64, 64])
    lam_s = cx.sb("lam_s", [64, 4])
    neglam = cx.sb("neglam", [64, 1])
    gs_sb = cx.sb("gs_sb", [64, 1])
    kb.dma("sp", lam_sb[:], lamp[0:1, :].broadcast_to([64, 128]), writes=["lam_sb"])
    kb.dma("sp", gs_sb[:], gsub[:, :], writes=["gs_sb"])
    for i in range(2):
        kb.op("dve", lambda e: e.tensor_tensor(out=lam_t[:, i * 32:(i + 1) * 32], in0=lam_sb[:, i * 64:i * 64 + 32],
                                               in1=lam_sb[:, i * 64 + 32:i * 64 + 64], op=ALU.mult),
              reads=["lam_sb"], writes=["lam_t"])
        kb.op("dve", lambda e: e.tensor_reduce(out=lam_s[:, i:i + 1], in_=lam_t[:, i * 32:(i + 1) * 32], axis=AX.X, op=ALU.add),
              reads=["lam_t"], writes=["lam_s"])
    kb.op("act", lambda e: e.activation(out=lam_s[:, 2:4], in_=lam_s[:, 0:2], func=AF.Exp), reads=["lam_s"], writes=["lam_s"])
    kb.op("dve", lambda e: e.tensor_tensor(out=neglam[:], in0=lam_s[:, 3:4], in1=lam_s[:, 2:3], op=ALU.subtract),
          reads=["lam_s"], writes=["neglam"])
    kb.op("dve", lambda e: e.tensor_scalar(out=neglam[:], in0=neglam[:], scalar1=-lam_init, scalar2=None, op0=ALU.add),
          reads=["neglam"], writes=["neglam"])
    kb.op("dve", lambda e: e.tensor_scalar(out=gs_sb[:], in0=gs_sb[:], scalar1=1.0 - lam_init, scalar2=None, op0=ALU.mult),
          reads=["gs_sb"], writes=["gs_sb"])

    with contextlib.ExitStack() as ph:
        ksb = [cx.sb(f"ksb{i}", [128, NKEY], BF16, stack=ph) for i in range(2)]
        vsb = [cx.sb(f"vsb{i}", [128, NKT, 65], BF16, stack=ph) for i in range(2)]
        qsb = [cx.sb(f"qsb{i}", [128, NTOK], BF16, stack=ph) for i in range(2)]
        pT = [cx.sb(f"pT{i}", [128, 2, 512], BF16, stack=ph) for i in range(2)]
        osb = cx.sb("osb", [65, 512], stack=ph)
        rec = cx.sb("rec", [64, 512], stack=ph)
        a1 = cx.sb("a1", [64, NTOK], stack=ph)
        dd = cx.sb("dd", [64, 512], stack=ph)
        sqd = cx.sb("sqd", [64, 512], stack=ph)
        rsd = cx.sb("rsd", [64, 512], stack=ph)
        ob = [cx.sb(f"ob{i}", [64, 512], BF16, stack=ph) for i in range(2)]
        knas = cx.sb("knas", [64, NA_T * 128], BF16, stack=ph)
        vnas = cx.sb("vnas", [128, NA_T, 65], BF16, stack=ph)
        nabs = cx.sb("nabs", [128, 5, 7 * 128], stack=ph)
        tl = cx.sb("tl", [128, 7 * 128], stack=ph)
        pna = cx.sb("pna", [128, 9 * 128], BF16, stack=ph)
        sps = [cx.ps(f"sps{i}", [128, 2, 512], stack=ph) for i in range(2)]
        ops = [cx.ps(f"ops{i}", [128, 512], stack=ph) for i in range(2)]
        pbc = cx.ps("pbc", [128, 512], stack=ph)
        cnt = {"pair": 0, "blk": 0, "ob": 0}

        def finalize(opsi, nq, kind, orow, tok0):
            okey = ("ops", opsi)
            kb.op("act", lambda e: e.copy(out=osb[:, 0:nq], in_=ops[opsi][0:65, 0:nq]), reads=[okey], writes=["osb"])
            kb.op("pe", lambda e: e.matmul(pbc[0:64, 0:nq], lhsT=sel65[0:65, :], rhs=osb[0:65, 0:nq], start=True, stop=True),
                  reads=["osb", "sel65"], writes=["pbc"])
            kb.op("dve", lambda e: e.reciprocal(out=rec[:, 0:nq], in_=pbc[0:64, 0:nq]), reads=["pbc"], writes=["rec"])
            if kind == "diff0":
                kb.op("dve", lambda e: e.tensor_tensor(out=a1[:, tok0:tok0 + nq], in0=osb[0:64, 0:nq], in1=rec[:, 0:nq], op=ALU.mult),
                      reads=["osb", "rec"], writes=[("a1", tok0)])
                return
            oi = cnt["ob"] % 2
            cnt["ob"] += 1
            if kind == "plain":
                kb.op("dve", lambda e: e.tensor_tensor(out=ob[oi][:, 0:nq], in0=osb[0:64, 0:nq], in1=rec[:, 0:nq], op=ALU.mult),
                      reads=["osb", "rec"], writes=[("ob", oi)])
            else:
                kb.op("dve", lambda e: e.tensor_tensor(out=dd[:, 0:nq], in0=osb[0:64, 0:nq], in1=rec[:, 0:nq], op=ALU.mult),
                      reads=["osb", "rec"], writes=["dd"])
                kb.op("dve", lambda e: e.scalar_tensor_tensor(out=dd[:, 0:nq], in0=dd[:, 0:nq], scalar=neglam[:, 0:1], in1=a1[:, tok0:tok0 + nq],
                                                              op0=ALU.mult, op1=ALU.add),
                      reads=["dd", "neglam", ("a1", tok0)], writes=["dd"])
                kb.op("pool", lambda e: e.tensor_tensor(out=sqd[:, 0:nq], in0=dd[:, 0:nq], in1=dd[:, 0:nq], op=ALU.mult),
                      reads=["dd"], writes=["sqd"])
                kb.op("pe", lambda e: e.matmul(pbc[0:64, 0:nq], lhsT=ones64[:, :], rhs=sqd[:, 0:nq], start=True, stop=True),
                      reads=["sqd", "ones64"], writes=["pbc"])
                kb.op("act", lambda e: e.activation(out=rsd[:, 0:nq], in_=pbc[0:64, 0:nq], func=AF.Sqrt, scale=1.0 / 64, bias=eps[0:64, 0:1]),
                      reads=["pbc", "eps"], writes=["rsd"])
                kb.op("dve", lambda e: e.reciprocal(out=rsd[:, 0:nq], in_=rsd[:, 0:nq]), reads=["rsd"], writes=["rsd"])
                kb.op("dve", lambda e: e.scalar_tensor_tensor(out=ob[oi][:, 0:nq], in0=dd[:, 0:nq], scalar=gs_sb[:, 0:1], in1=rsd[:, 0:nq],
                                                              op0=ALU.mult, op1=ALU.mult),
                      reads=["dd", "gs_sb", "rsd"], writes=[("ob", oi)])
            kb.dma("sp" if oi == 0 else "pool", oT_d[orow:orow + 64, tok0:tok0 + nq], ob[oi][:, 0:nq],
                   reads=[("ob", oi)], writes=["oT_d"], slot=("obo", oi))

        def attend(kt_ap_fn, kkey, v_ap_fn, vkey, q_ap, qkey, dk, ktiles, nq, scale, kind, orow, tok0):
            opsi = cnt["blk"] % 2
            cnt["blk"] += 1
            pairs = [(ktiles[2 * i], ktiles[2 * i + 1]) for i in range(len(ktiles) // 2)]
            npair = len(pairs)
            base = cnt["pair"]
            cnt["pair"] += npair

            def emit_S(p):
                si = (base + p) % 2
                for j, kt in enumerate(pairs[p]):
                    kb.op("pe", lambda e: e.matmul(sps[si][:, j, 0:nq], lhsT=kt_ap_fn(kt), rhs=q_ap, start=True, stop=True),
                          reads=list(kkey) + list(qkey), writes=[("sps", si, j)])

            emit_S(0)
            for p in range(npair):
                si = (base + p) % 2
                if p + 1 < npair:
                    emit_S(p + 1)
                for j in range(2):
                    kb.op("act", lambda e: e.activation(out=pT[si][:, j, 0:nq], in_=sps[si][:, j, 0:nq], func=AF.Exp, scale=float(scale)),
                          reads=[("sps", si, j)], writes=[("pT", si, j)])
                for j, kt in enumerate(pairs[p]):
                    kb.op("pe", lambda e: e.matmul(ops[opsi][0:65, 0:nq], lhsT=v_ap_fn(kt), rhs=pT[si][:, j, 0:nq],
                                                   start=(p == 0 and j == 0), stop=(p == npair - 1 and j == 1)),
                          reads=list(vkey) + [("pT", si, j)], writes=[("ops", opsi)])
            finalize(opsi, nq, kind, orow, tok0)

        heads = _full_heads()
        vA_v = vA.rearrange("t p w -> p t w")

        def load_head(hi):
            hd = heads[hi]
            s = hi % 2
            p0 = 0
            for i, (r0, n) in enumerate(hd["k"]):
                kb.dma("sp", ksb[s][p0:p0 + n, :], kT[r0:r0 + n, :], writes=[("ksb", s, i)], slot=("ksb", s))
                p0 += n
            if len(hd["k"]) == 1:
                kb.last_w[("ksb", s, 1)] = kb.last_w[("ksb", s, 0)]
                kb.readers[("ksb", s, 1)] = []
            p0 = 0
            for (r0, n) in hd["q"]:
                kb.dma("sp", qsb[s][p0:p0 + n, :], qT[r0:r0 + n, :], writes=[("qsb", s)], slot=("qsb", s))
                p0 += n
            vh = hd["vh"]
            for part in range(5):
                t0, t1 = part * 26, (part + 1) * 26
                kb.dma("pool", vsb[s][:, t0:t1, :], vA_v[:, t0:t1, vh * 65:(vh + 1) * 65], writes=[("vsb", s, part)], slot=("vsb", s))

        if "f" not in parts:
            heads = []
        heads = heads[:nheads]
        if heads:
            load_head(0)
        all_kt = list(range(NKT))
        for hi, hd in enumerate(heads):
            s = hi % 2
            if hi + 1 < len(heads):
                load_head(hi + 1)
            dk = hd["dk"]
            for qb in range(NT // 512):
                attend(lambda kt: ksb[s][0:dk, kt * 128:(kt + 1) * 128], [("ksb", s, 0), ("ksb", s, 1)],
                       lambda kt: vsb[s][:, kt, :], [("vsb", s, i) for i in range(5)],
                       qsb[s][0:dk, qb * 512:(qb + 1) * 512], [("qsb", s)], dk, all_kt, 512, hd["scale"], hd["kind"],
                       hd["orow"], qb * 512)
            if need_ctx:
                attend(lambda kt: ksb[s][0:dk, kt * 128:(kt + 1) * 128], [("ksb", s, 0), ("ksb", s, 1)],
                       lambda kt: vsb[s][:, kt, :], [("vsb", s, i) for i in range(5)],
                       qsb[s][0:dk, NT:NTOK], [("qsb", s)], dk, [0, 1], NCTX, hd["scale"], hd["kind"], hd["orow"], NT)

        vna_v = vna.rearrange("t p w -> p t w")
        sna = [sps[i][:].rearrange("p a b -> p (a b)") for i in range(2)]
        for h in range(4 if "n" in parts else 0):
            kb.dma("sp", knas[:, :], kna[64 * h:64 * (h + 1), :], writes=["knas"], slot=("knas",))
            kb.dma("sp", qsb[0][0:64, :], qT[64 * h:64 * (h + 1), :], writes=[("qsb", 0)], slot=("qsb", 0))
            kb.dma("pool", vnas[:], vna_v[:, :, h * 65:(h + 1) * 65], writes=["vnas"], slot=("vnas",))
            kb.dma("pool", nabs[:].rearrange("p a b -> p (a b)"), nab[h, :, :], writes=["nabs"], slot=("nabs",))
            for i in range(NT // 128):
                slot_i = {0: 0, 1: 1, 30: 3, 31: 4}.get(i, 2)
                for a in range(7):
                    ktile = 2 + i + a
                    kb.op("pe", lambda e: e.matmul(sna[0][:, a * 128:(a + 1) * 128], lhsT=knas[0:64, ktile * 128:(ktile + 1) * 128],
                                                   rhs=qsb[0][0:64, i * 128:(i + 1) * 128], start=True, stop=True),
                          reads=["knas", ("qsb", 0)], writes=[("sps", 0, 0), ("sps", 0, 1)])
                for a in range(2):
                    kb.op("pe", lambda e: e.matmul(sna[1][:, a * 128:(a + 1) * 128], lhsT=knas[0:64, a * 128:(a + 1) * 128],
                                                   rhs=qsb[0][0:64, i * 128:(i + 1) * 128], start=True, stop=True),
                          reads=["knas", ("qsb", 0)], writes=[("sps", 1, 0)])
                for (c0, c1) in ((0, 512), (512, 896)):
                    kb.op("dve", lambda e: e.scalar_tensor_tensor(out=tl[:, c0:c1], in0=sna[0][:, c0:c1], scalar=0.125,
                                                                  in1=nabs[:, slot_i, c0:c1], op0=ALU.mult, op1=ALU.add),
                          reads=[("sps", 0, 0), ("sps", 0, 1), "nabs"], writes=[("tl", c0)])
                kb.op("act", lambda e: e.activation(out=pna[:, 0:896], in_=tl[:], func=AF.Exp), reads=[("tl", 0), ("tl", 512)], writes=[("pna", 0)])
                kb.op("act", lambda e: e.activation(out=pna[:, 896:1152], in_=sna[1][:, 0:256], func=AF.Exp, scale=0.125),
                      reads=[("sps", 1, 0)], writes=[("pna", 1)])
                col = (i % 4) * 128
                for a in range(9):
                    ktile = (2 + i + a) if a < 7 else (a - 7)
                    kb.op("pe", lambda e: e.matmul(ops[0][0:65, col:col + 128], lhsT=vnas[:, ktile, :], rhs=pna[:, a * 128:(a + 1) * 128],
                                                   start=(a == 0), stop=(a == 8)),
                          reads=["vnas", ("pna", 0), ("pna", 1)], writes=[("ops", 0)])
                if i % 4 == 3:
                    finalize(0, 512, "plain", 64 * h, (i // 4) * 512)
            if need_ctx:
                attend(lambda kt: knas[0:64, kt * 128:(kt + 1) * 128], ["knas"], lambda kt: vnas[:, kt, :], ["vnas"],
                       qsb[0][0:64, NT:NTOK], [("qsb", 0)], 64, [0, 1], NCTX, 0.125, "plain", 64 * h, NT)
        cx.barrier()

    with contextlib.ExitStack() as ph:
        wout_bf = cx.sb("wout_bf", [128, 8, D], BF16, stack=ph)
        wst = [cx.sb(f"wst{i}", [128, D], stack=ph) for i in range(2)]
        G1 = cx.sb("G1", [128, 2, D], stack=ph)
        A2 = cx.sb("A2", [128, 2, D], stack=ph)
        B2 = cx.sb("B2", [128, 2, D], stack=ph)
        gb = cx.sb("gb", [128, D], stack=ph)
        wr_sb = cx.sb("wr_sb", [128, 8, NE], stack=ph)
        oT = [cx.sb(f"oT{i}", [128, 8, 128], BF16, stack=ph) for i in range(2)]
        xt = [cx.sb(f"xt{i}", [128, D], stack=ph) for i in range(2)]
        x1 = [cx.sb(f"x1{i}", [128, D], stack=ph) for i in range(2)]
        tmp = cx.sb("tmp", [128, D], stack=ph)
        junk = cx.sb("junk", [128, D], stack=ph)
        h2 = cx.sb("h2", [128, D], stack=ph)
        h2Tf = cx.sb("h2Tf", [128, D], stack=ph)
        sH = [cx.sb(f"sH{i}", [128, 8, 512], BF16, stack=ph) for i in range(2)]
        st_ss = cx.sb("st_ss", [128, 1], stack=ph)
        st_rstd = cx.sb("st_rstd", [128, 1], stack=ph)
        lg = cx.sb("lg", [128, NE], stack=ph)
        mx = cx.sb("mx", [128, 1], stack=ph)
        sm = cx.sb("sm", [128, 1], stack=ph)
        af = [cx.sb(f"af{i}", [128, NE], stack=ph) for i in range(2)]
        py = cx.ps("py", [128, 2, 512], stack=ph)
        ptr = cx.ps("ptr", [128, D], stack=ph)
        plog = cx.ps("plog", [128, 512], stack=ph)
        for c in range(8):
            s = c % 2
            kb.dma("sp" if s == 0 else "pool", wst[s][:], wout[c * 128:(c + 1) * 128, :], writes=[("wst", s)], slot=("wstA", s))
            kb.op("dve" if s == 0 else "pool", lambda e: e.tensor_copy(out=wout_bf[:, c, :], in_=wst[s][:]), reads=[("wst", s)], writes=["wout_bf"])
        kb.dma("sp", wr_sb[:], wr.rearrange("(c p) e -> p c e", p=128), writes=["wr_sb"])
        kb.dma("sp", gb[:], gffn[0:1, :].broadcast_to([128, D]), writes=["gb"])
        for m in range(2):
            kb.dma("sp", G1[:, m, :], modv[m:m + 1, 2 * D:3 * D].broadcast_to([128, D]), writes=[("G1", m)])
            kb.dma("pool", B2[:, m, :], modv[m:m + 1, 3 * D:4 * D].broadcast_to([128, D]), writes=[("B2", m)])
            kb.dma("sp", tmp[:], modv[m:m + 1, 4 * D:5 * D].broadcast_to([128, D]), writes=["tmp"])
            kb.op("dve", lambda e: e.scalar_tensor_tensor(out=A2[:, m, :], in0=tmp[:], scalar=1.0, in1=gb[:], op0=ALU.add, op1=ALU.mult),
                  reads=["tmp", "gb"], writes=[("A2", m)])
        oT_v = oT_d.rearrange("(c p) t -> p c t", p=128)
        h2T_v = h2T_o.rearrange("(c p) t -> p c t", p=128)
        for t in range(NQT if "p" in parts else 0):
            m = 0 if t < NT // 128 else 1
            s = t % 2
            grp = t // 4
            gs = grp % 2
            tin = t % 4
            dq = "sp" if s == 0 else "pool"
            kb.dma(dq, oT[s][:], oT_v[:, :, t * 128:(t + 1) * 128], reads=["oT_d"], writes=[("oT", s)], slot=("oT", s))
            kb.dma(dq, xt[s][:], xin[t * 128:(t + 1) * 128, :], writes=[("xt", s)], slot=("xtA", s))
            for cb in range(2):
                for c in range(8):
                    kb.op("pe", lambda e: e.matmul(py[:, cb, :], lhsT=oT[s][:, c, :], rhs=wout_bf[:, c, cb * 512:(cb + 1) * 512],
                                                   start=(c == 0), stop=(c == 7)),
                          reads=[("oT", s), "wout_bf"], writes=[("py", cb)])
            for cb in range(2):
                kb.op("dve", lambda e: e.tensor_tensor(out=tmp[:, cb * 512:(cb + 1) * 512], in0=py[:, cb, :], in1=G1[:, m, cb * 512:(cb + 1) * 512], op=ALU.mult),
                      reads=[("py", cb), ("G1", m)], writes=[("tmp", cb)])
            kb.op("pool", lambda e: e.tensor_tensor(out=x1[s][:], in0=tmp[:], in1=xt[s][:], op=ALU.add),
                  reads=[("tmp", 0), ("tmp", 1), ("xt", s)], writes=[("x1", s), "tmp"])
            kb.dma(dq, x1_o[t * 128:(t + 1) * 128, :], x1[s][:], reads=[("x1", s)], writes=["x1_d"], slot=("x1o", s))
            kb.op("act", lambda e: e.activation(out=junk[:], in_=x1[s][:], func=AF.Square, accum_out=st_ss[:]),
                  reads=[("x1", s)], writes=["junk", "st_ss"])
            rstd_from_ss(cx, st_ss[:], st_rstd[:], D, eps[:, 0:1], ["st_ss"], "st_rstd")
            kb.op("dve", lambda e: e.scalar_tensor_tensor(out=tmp[:], in0=x1[s][:], scalar=st_rstd[:, 0:1], in1=A2[:, m, :],
                                                          op0=ALU.mult, op1=ALU.mult),
                  reads=[("x1", s), "st_rstd", ("A2", m)], writes=["tmp", ("tmp", 0), ("tmp", 1)])
            kb.op("pool", lambda e: e.tensor_tensor(out=h2[:], in0=tmp[:], in1=B2[:, m, :], op=ALU.add),
                  reads=["tmp", ("B2", m)], writes=["h2"])
            for c in range(8):
                kb.op("pe", lambda e: e.transpose(out=ptr[:, c * 128:(c + 1) * 128], in_=h2[:, c * 128:(c + 1) * 128], identity=identf[:]),
                      reads=["h2", "ident_f"], writes=[("ptr", c)])
            for hb_ in range(2):
                kb.op("act", lambda e: e.copy(out=h2Tf[:, hb_ * 512:(hb_ + 1) * 512], in_=ptr[:, hb_ * 512:(hb_ + 1) * 512]),
                      reads=[("ptr", c) for c in range(4 * hb_, 4 * hb_ + 4)], writes=[("h2Tf", hb_)])
                kb.op("dve", lambda e: e.tensor_copy(out=sH[gs][:, 4 * hb_:4 * hb_ + 4, tin * 128:(tin + 1) * 128],
                                                     in_=h2Tf[:, hb_ * 512:(hb_ + 1) * 512].rearrange("p (c w) -> p c w", w=128)),
                      reads=[("h2Tf", hb_)], writes=[("sH", gs)])
            for c in range(8):
                kb.op("pe", lambda e: e.matmul(plog[:, 0:NE], lhsT=h2Tf[:, c * 128:(c + 1) * 128], rhs=wr_sb[:, c, :], start=(c == 0), stop=(c == 7)),
                      reads=[("h2Tf", 0), ("h2Tf", 1), "wr_sb"], writes=["plog"])
            kb.op("dve", lambda e: e.tensor_reduce(out=mx[:], in_=plog[:, 0:NE], axis=AX.X, op=ALU.max), reads=["plog"], writes=["mx"])
            kb.op("dve", lambda e: e.tensor_scalar(out=mx[:], in0=mx[:], scalar1=-1.0, scalar2=None, op0=ALU.mult), reads=["mx"], writes=["mx"])
            kb.op("act", lambda e: e.activation(out=lg[:], in_=plog[:, 0:NE], func=AF.Exp, bias=mx[:, 0:1], accum_out=sm[:]),
                  reads=["plog", "mx"], writes=["lg", "sm"])
            kb.op("dve", lambda e: e.reciprocal(out=sm[:], in_=sm[:]), reads=["sm"], writes=["sm"])
            kb.op("dve", lambda e: e.tensor_scalar(out=af[s][:], in0=lg[:], scalar1=sm[:, 0:1], scalar2=None, op0=ALU.mult),
                  reads=["lg", "sm"], writes=[("af", s)])
            kb.dma(dq, aff_o[t * 128:(t + 1) * 128, :], af[s][:], reads=[("af", s)], writes=["aff_d"], slot=("afo", s))
            if tin == 3 or t == NQT - 1:
                ntk = (tin + 1) * 128
                kb.dma("sp", h2T_v[:, :, grp * 512:grp * 512 + ntk], sH[gs][:, :, 0:ntk], reads=[("sH", gs)], writes=["h2T_d"], slot=("sHo", gs))
        cx.barrier()
    return cx.finish()


def _na_bias_tables(rpb, r):
    H = 4
    rows = SEQ // 64
    out = np.full((H, 5, 128, 7, 128), -30000.0, np.float32)
    kp = np.arange(128)
    qp = np.arange(128)
    for si, i_loc in enumerate((0, 1, 5, 30, 31)):
        j = 32 * r + i_loc
        qr = 2 * j + qp // 64
        qc = qp % 64
        r0 = np.clip(qr - 4, 0, rows - 8)
        c0 = np.clip(qc - 8, 0, 64 - 16)
        for t in range(7):
            g = j - 3 + t
            if g < 0 or g >= 128:
                continue
            kr = 2 * g + kp // 64
            kc = kp % 64
            valid = ((kr[:, None] >= r0[None, :]) & (kr[:, None] < r0[None, :] + 8) &
                     (kc[:, None] >= c0[None, :]) & (kc[:, None] < c0[None, :] + 16))
            ri = np.clip(kr[:, None] - qr[None, :] + 7, 0, 14)
            ci = np.clip(kc[:, None] - qc[None, :] + 15, 0, 30)
            for h in range(H):
                vals = rpb[h][ri, ci]
                out[h, si, :, t, :] = np.where(valid, vals, np.float32(-30000.0))
    return np.ascontiguousarray(out.transpose(0, 2, 1, 3, 4).reshape(H, 128, 5 * 7 * 128))


def run_A(inp, l, resP, x_cur, xc_cur, **kw):
    nc = _get_nc("A", build_A, l) if not kw else build_A(l, **kw)
    in_maps = []
    zk = None
    for c in range(NCORE):
        b, r = c // 4, c % 4
        grp = [resP[4 * b + rr] for rr in range(4)]
        kT_all = np.concatenate([resP[c]["kT"][:, NT:NTOK]] + [g["kT"][:, 0:NT] for g in grp], axis=1)
        v_all = np.concatenate([resP[c]["v"][32:34]] + [g["v"][0:32] for g in grp], axis=0)
        kna = np.zeros((256, NA_T * 128), kT_all.dtype)
        vna = np.zeros((NA_T, 128, 4 * 65), v_all.dtype)
        kna[:, 0:256] = kT_all[0:256, 0:256]
        vna[0:2] = v_all[0:2, :, 0:260]
        for i in range(32 + 2 * NA_HALO):
            g = 32 * r + i - NA_HALO
            if 0 <= g < 128:
                kna[:, (2 + i) * 128:(3 + i) * 128] = kT_all[0:256, 256 + g * 128:256 + (g + 1) * 128]
                vna[2 + i] = v_all[2 + g, :, 0:260]
        in_maps.append({
            "qT": resP[c]["qT"], "kT": np.ascontiguousarray(kT_all), "v": np.ascontiguousarray(v_all),
            "kna": kna, "vna": vna, "nab": _na_bias_tables(np.asarray(inp["na_rpb"][l], np.float32), r),
            "xin": np.ascontiguousarray(np.concatenate([x_cur[b, r * NT:(r + 1) * NT], xc_cur[b]], axis=0)),
            "wout": np.ascontiguousarray(inp["w_out"][l]),
            "modv": resP[c]["modv"],
            "gffn": np.ascontiguousarray(inp["g_ffn"][l][None, :]),
            "wr": np.ascontiguousarray(inp["w_router"][l]),
            "gsub": np.ascontiguousarray(inp["g_diff_sub"][l].reshape(64, 1)),
            "lamp": np.ascontiguousarray(inp["diff_lambda"][l].reshape(1, 128)),
        })
    res = run_bass_kernel_spmd(nc, in_maps, core_ids=list(range(NCORE)))
    return res.results, in_maps


def _e_parts(l):
    return [(0, 12), (12, 12), (24, 10)] if l == 0 else [(0, 11), (11, 11), (22, 10)]


def build_E(l, n_exp=NE, n_jp=DFF // 256):
    need_ctx = (l == 0)
    NQ = NTOK if need_ctx else NT
    cx = Ctx()
    nc, kb = cx.nc, cx.kb
    h2T = cx.din("h2T", [D, NQ], BF16)
    affA = cx.din("affA", [NE, SEQ])
    affC = cx.din("affC", [NE, NCTX])
    affO = cx.din("affO", [NE, NQ])
    x1 = cx.din("x1", [NQ, D])
    modv = cx.din("modv", [2, 6 * D])
    wg = cx.din("wg", [NE, D, DFF])
    wu = cx.din("wu", [NE, D, DFF])
    wd = cx.din("wd", [NE, DFF, D])
    x2_o = cx.dout("x2", [NQ, D])

    identf, identb = make_ident(cx)
    gsel = cx.sb("gsel", [NE, NQ])
    thr = cx.sb("thr", [NE, 2])

    with contextlib.ExitStack() as ph:
        aall = cx.sb("aall", [NE, SEQ], stack=ph)
        jk = cx.sb("jk", [NE, SEQ], BF16, stack=ph)
        actx = cx.sb("actx", [NE, NCTX], stack=ph)
        aown = cx.sb("aown", [NE, NQ], stack=ph)
        msk = cx.sb("msk", [NE, NQ], stack=ph)
        lo = cx.sb("lo", [NE, 1], stack=ph)
        hi = cx.sb("hi", [NE, 1], stack=ph)
        mid = cx.sb("mid", [NE, 1], stack=ph)
        cn = cx.sb("cn", [NE, 1], stack=ph)
        ge = cx.sb("ge", [NE, 1], stack=ph)
        d1 = cx.sb("d1", [NE, 1], stack=ph)
        d2 = cx.sb("d2", [NE, 1], stack=ph)
        for q in range(4):
            kb.dma("sp" if q % 2 == 0 else "pool", aall[:, q * 4096:(q + 1) * 4096], affA[:, q * 4096:(q + 1) * 4096], writes=[("aall", q)])
        kb.dma("sp", actx[:], affC[:, :], writes=["actx"])
        kb.dma("sp", aown[:], affO[:, :], writes=["aown"])
        AALL = [("aall", q) for q in range(4)]

        def bisect(data_ap, n, cap, rkeys, col):
            kb.op("dve", lambda e: e.memset(lo[:], 0.0), writes=["lo"])
            kb.op("dve", lambda e: e.memset(hi[:], 1.0), writes=["hi"])
            for it in range(40):
                kb.op("dve", lambda e: e.tensor_tensor(out=mid[:], in0=lo[:], in1=hi[:], op=ALU.add), reads=["lo", "hi"], writes=["mid"])
                kb.op("dve", lambda e: e.tensor_scalar(out=mid[:], in0=mid[:], scalar1=0.5, scalar2=None, op0=ALU.mult), reads=["mid"], writes=["mid"])
                kb.op("dve", lambda e: e.tensor_scalar(out=jk[:, 0:n], in0=data_ap, scalar1=mid[:, 0:1], scalar2=None, op0=ALU.is_gt, op1=ALU.add,
                                                       accum_out=cn[:, 0:1]), reads=list(rkeys) + ["mid"], writes=["jk", "cn"])
                kb.op("dve", lambda e: e.tensor_scalar(out=ge[:], in0=cn[:], scalar1=float(cap) - 0.5, scalar2=None, op0=ALU.is_gt), reads=["cn"], writes=["ge"])
                kb.op("dve", lambda e: e.tensor_tensor(out=d1[:], in0=mid[:], in1=lo[:], op=ALU.subtract), reads=["mid", "lo"], writes=["d1"])
                kb.op("dve", lambda e: e.tensor_tensor(out=d2[:], in0=hi[:], in1=mid[:], op=ALU.subtract), reads=["mid", "hi"], writes=["d2"])
                kb.op("dve", lambda e: e.scalar_tensor_tensor(out=lo[:], in0=d1[:], scalar=ge[:, 0:1], in1=lo[:], op0=ALU.mult, op1=ALU.add),
                      reads=["d1", "ge", "lo"], writes=["lo"])
                kb.op("dve", lambda e: e.scalar_tensor_tensor(out=hi[:], in0=d2[:], scalar=ge[:, 0:1], in1=mid[:], op0=ALU.mult, op1=ALU.add),
                      reads=["d2", "ge", "mid"], writes=["hi"])
            kb.op("dve", lambda e: e.tensor_copy(out=thr[:, col:col + 1], in_=lo[:]), reads=["lo"], writes=[("thr", col)])

        bisect(aall[:, :], SEQ, 2 * SEQ // NE, AALL, 0)
        if need_ctx:
            bisect(actx[:, :], NCTX, 2 * NCTX // NE, ["actx"], 1)
        segs = [(0, NT, 0)] + ([(NT, NTOK, 1)] if need_ctx else [])
        for (c0, c1, col) in segs:
            kb.op("dve", lambda e: e.tensor_scalar(out=msk[:, c0:c1], in0=aown[:, c0:c1], scalar1=thr[:, col:col + 1], scalar2=None, op0=ALU.is_gt),
                  reads=["aown", ("thr", col)], writes=[("msk", col)])
            kb.op("dve", lambda e: e.tensor_tensor(out=gsel[:, c0:c1], in0=msk[:, c0:c1], in1=aown[:, c0:c1], op=ALU.mult),
                  reads=[("msk", col), "aown"], writes=["gsel"])
        cx.barrier()

    MAXT = 12 * 128
    oh = cx.sb("oh", [NE, NE, 128])
    kb.op("pool", lambda e: e.memset(oh[:], 1.0), writes=["oh"])
    kb.op("pool", lambda e: e.affine_select(out=oh[:], in_=oh[:], pattern=[[-1, NE], [0, 128]], compare_op=ALU.is_equal, fill=0.0,
                                            base=0, channel_multiplier=1), reads=["oh"], writes=["oh"])
    G2 = cx.sb("G2", [128, 2, D])
    for m in range(2):
        kb.dma("sp", G2[:, m, :], modv[m:m + 1, 5 * D:6 * D].broadcast_to([128, D]), writes=[("G2", m)])
    hsb = cx.sb("hsb", [128, 8, MAXT], BF16)
    yacc = cx.sb("yacc", [128, 8, MAXT])
    gbc = cx.sb("gbc", [128, MAXT])
    stg = cx.sb("stg", [128, 8, 256])
    stu = cx.sb("stu", [128, 8, 256])
    std = cx.sb("std", [128, 2, D])
    wgb = [cx.sb(f"wgb{i}", [128, 8, 256], BF16) for i in range(2)]
    wub = [cx.sb(f"wub{i}", [128, 8, 256], BF16) for i in range(2)]
    wdb = [cx.sb(f"wdb{i}", [128, 2, D], BF16) for i in range(2)]
    sg = cx.sb("sg", [128, 2, 512])
    ug = cx.sb("ug", [128, 2, 512])
    act = [cx.sb(f"act{i}", [128, 2, 512], BF16) for i in range(2)]
    xt = cx.sb("xt", [128, D])
    tmp = cx.sb("tmp", [128, D])
    x2 = [cx.sb(f"x2{i}", [128, D]) for i in range(2)]
    gps = cx.ps("gps", [128, 2, 512])
    ups = cx.ps("ups", [128, 2, 512])
    yps = [cx.ps(f"yps{i}", [128, 512]) for i in range(3)]
    pgb = cx.ps("pgb", [128, 512])
    h2T_v = h2T.rearrange("(c p) t -> p c t", p=128)
    cnt = {"w": 0, "y": 0, "a": 0}

    for (t0, ntl) in _e_parts(l):
        tok0, ntok = t0 * 128, ntl * 128
        blocks = [(b, min(512, ntok - b)) for b in range(0, ntok, 512)]
        kb.dma("sp", hsb[:, :, 0:ntok], h2T_v[:, :, tok0:tok0 + ntok], writes=["hsb"], slot=("hsb",))
        kb.op("pool", lambda e: e.memset(yacc[:], 0.0), writes=[("yacc", dc) for dc in range(8)])
        for ex in range(n_exp):
            for (b0, n) in blocks:
                kb.op("pe", lambda e: e.matmul(pgb[:, 0:n], lhsT=oh[:, ex, :], rhs=gsel[:, tok0 + b0:tok0 + b0 + n], start=True, stop=True),
                      reads=["oh", "gsel"], writes=["pgb"])
                kb.op("act", lambda e: e.copy(out=gbc[:, b0:b0 + n], in_=pgb[:, 0:n]), reads=["pgb"], writes=["gbc"])
            for jp in range(n_jp):
                ws = cnt["w"] % 2
                cnt["w"] += 1
                f0 = jp * 256
                kb.dma("sp", stg[:], wg[ex].rearrange("(c p) f -> p c f", p=128)[:, :, f0:f0 + 256], writes=["stg"], slot=("stg",))
                kb.dma("sp", stu[:], wu[ex].rearrange("(c p) f -> p c f", p=128)[:, :, f0:f0 + 256], writes=["stu"], slot=("stu",))
                kb.dma("pool", std[:], wd[ex, f0:f0 + 256, :].rearrange("(j p) d -> p j d", p=128), writes=["std"], slot=("std",))
                kb.op("pool", lambda e: e.tensor_copy(out=wgb[ws][:], in_=stg[:]), reads=["stg"], writes=[("wgb", ws)])
                kb.op("pool", lambda e: e.tensor_copy(out=wub[ws][:], in_=stu[:]), reads=["stu"], writes=[("wub", ws)])
                kb.op("pool", lambda e: e.tensor_copy(out=wdb[ws][:], in_=std[:]), reads=["std"], writes=[("wdb", ws)])
                for (b0, n) in blocks:
                    ai = cnt["a"] % 2
                    cnt["a"] += 1
                    for jj in range(2):
                        for c in range(8):
                            kb.op("pe", lambda e: e.matmul(gps[:, jj, 0:n], lhsT=wgb[ws][:, c, jj * 128:(jj + 1) * 128], rhs=hsb[:, c, b0:b0 + n],
                                                           start=(c == 0), stop=(c == 7)),
                                  reads=[("wgb", ws), "hsb"], writes=[("gps", jj)])
                        for c in range(8):
                            kb.op("pe", lambda e: e.matmul(ups[:, jj, 0:n], lhsT=wub[ws][:, c, jj * 128:(jj + 1) * 128], rhs=hsb[:, c, b0:b0 + n],
                                                           start=(c == 0), stop=(c == 7)),
                                  reads=[("wub", ws), "hsb"], writes=[("ups", jj)])
                    for jj in range(2):
                        kb.op("act", lambda e: e.activation(out=sg[:, jj, 0:n], in_=gps[:, jj, 0:n], func=AF.Silu), reads=[("gps", jj)], writes=[("sg", jj)])
                        kb.op("dve", lambda e: e.tensor_tensor(out=ug[:, jj, 0:n], in0=ups[:, jj, 0:n], in1=gbc[:, b0:b0 + n], op=ALU.mult),
                              reads=[("ups", jj), "gbc"], writes=[("ug", jj)])
                        kb.op("pool", lambda e: e.tensor_tensor(out=act[ai][:, jj, 0:n], in0=sg[:, jj, 0:n], in1=ug[:, jj, 0:n], op=ALU.mult),
                              reads=[("sg", jj), ("ug", jj)], writes=[("act", ai, jj)])
                    for dc in range(8):
                        yi = cnt["y"] % 3
                        cnt["y"] += 1
                        for jj in range(2):
                            kb.op("pe", lambda e: e.matmul(yps[yi][:, 0:n], lhsT=wdb[ws][:, jj, dc * 128:(dc + 1) * 128], rhs=act[ai][:, jj, 0:n],
                                                           start=(jj == 0), stop=(jj == 1)),
                                  reads=[("wdb", ws), ("act", ai, jj)], writes=[("yps", yi)])
                        kb.op("dve", lambda e: e.tensor_tensor(out=yacc[:, dc, b0:b0 + n], in0=yps[yi][:, 0:n], in1=yacc[:, dc, b0:b0 + n], op=ALU.add),
                              reads=[("yps", yi), ("yacc", dc)], writes=[("yacc", dc)])
        for tt in range(ntl):
            t = t0 + tt
            m = 0 if t < NT // 128 else 1
            s = tt % 2
            kb.dma("sp", xt[:], x1[t * 128:(t + 1) * 128, :], writes=["xt"], slot=("xtE",))
            for dc in range(8):
                kb.op("pe", lambda e: e.transpose(out=gps[:, dc // 4, (dc % 4) * 128:(dc % 4 + 1) * 128], in_=yacc[:, dc, tt * 128:(tt + 1) * 128],
                                                  identity=identf[:]),
                      reads=[("yacc", dc), "ident_f"], writes=[("gps", dc // 4)])
            for hb_ in range(2):
                kb.op("dve", lambda e: e.tensor_tensor(out=tmp[:, hb_ * 512:(hb_ + 1) * 512], in0=gps[:, hb_, :], in1=G2[:, m, hb_ * 512:(hb_ + 1) * 512], op=ALU.mult),
                      reads=[("gps", hb_), ("G2", m)], writes=[("tmp", hb_)])
            kb.op("pool", lambda e: e.tensor_tensor(out=x2[s][:], in0=tmp[:], in1=xt[:], op=ALU.add),
                  reads=[("tmp", 0), ("tmp", 1), "xt"], writes=[("x2", s)])
            kb.dma("sp", x2_o[t * 128:(t + 1) * 128, :], x2[s][:], reads=[("x2", s)], writes=["x2_d"], slot=("x2o", s))
    return cx.finish()


def run_E(inp, l, resA, modvs, **kw):
    nc = _get_nc("E", build_E, l) if not kw else build_E(l, **kw)
    need_ctx = (l == 0)
    in_maps = []
    wg = np.ascontiguousarray(inp["w_gate"][l])
    wu = np.ascontiguousarray(inp["w_up"][l])
    wd = np.ascontiguousarray(inp["w_down"][l])
    for c in range(NCORE):
        b, r = c // 4, c % 4
        affA = np.ascontiguousarray(np.concatenate([resA[4 * b + rr]["aff"][0:NT] for rr in range(4)], axis=0).T)
        affO = np.ascontiguousarray(resA[c]["aff"].T)
        affC = np.ascontiguousarray(resA[c]["aff"][NT:NTOK].T) if need_ctx else np.zeros((NE, NCTX), np.float32)
        in_maps.append({"h2T": resA[c]["h2T"], "affA": affA, "affC": affC, "affO": affO, "x1": resA[c]["x1"],
                        "modv": modvs[c], "wg": wg, "wu": wu, "wd": wd})
    res = run_bass_kernel_spmd(nc, in_maps, core_ids=list(range(NCORE)))
    return res.results, in_maps


NEC = 4
PART_T = 12


def build_E2(l):
    need_ctx = (l == 0)
    NB = NTOK if need_ctx else NT
    NBT = SEQ + (NCTX if need_ctx else 0)
    NALL = NBT
    cx = Ctx()
    nc, kb = cx.nc, cx.kb
    h2T = cx.din("h2T", [D, NALL], BF16)
    affM = cx.din("affM", [1, NE, SEQ])
    affC = cx.din("affC", [1, NE, NCTX])
    affS = cx.din("affS", [NE, NALL])
    wg = cx.din("wg", [NEC, D, DFF])
    wu = cx.din("wu", [NEC, D, DFF])
    wd = cx.din("wd", [NEC, DFF, D])
    part_o = cx.dout("part", [NALL, D], BF16)
    gsel_d = cx.dint("gsel_d", [NE, NALL])

    identf, identb = make_ident(cx)
    thr = cx.sb("thr", [NE, 4])

    with contextlib.ExitStack() as ph:
        aall = cx.sb("aall", [NE, SEQ], stack=ph)
        jk = cx.sb("jk", [NE, SEQ], BF16, stack=ph)
        actx = cx.sb("actx", [NE, NCTX], stack=ph)
        aown = cx.sb("aown", [NE, 4096], stack=ph)
        msk = cx.sb("msk", [NE, 4096], stack=ph)
        gs_t = cx.sb("gs_t", [NE, 4096], stack=ph)
        lo = cx.sb("lo", [NE, 1], stack=ph)
        hi = cx.sb("hi", [NE, 1], stack=ph)
        mid = cx.sb("mid", [NE, 1], stack=ph)
        cn = cx.sb("cn", [NE, 1], stack=ph)
        ge = cx.sb("ge", [NE, 1], stack=ph)
        d1 = cx.sb("d1", [NE, 1], stack=ph)
        d2 = cx.sb("d2", [NE, 1], stack=ph)

        def bisect(data_ap, n, cap, rkeys, col):
            kb.op("dve", lambda e: e.memset(lo[:], 0.0), writes=["lo"])
            kb.op("dve", lambda e: e.memset(hi[:], 1.0), writes=["hi"])
            for it in range(40):
                kb.op("dve", lambda e: e.tensor_tensor(out=mid[:], in0=lo[:], in1=hi[:], op=ALU.add), reads=["lo", "hi"], writes=["mid"])
                kb.op("dve", lambda e: e.tensor_scalar(out=mid[:], in0=mid[:], scalar1=0.5, scalar2=None, op0=ALU.mult), reads=["mid"], writes=["mid"])
                kb.op("dve", lambda e: e.tensor_scalar(out=jk[:, 0:n], in0=data_ap, scalar1=mid[:, 0:1], scalar2=None, op0=ALU.is_gt, op1=ALU.add,
                                                       accum_out=cn[:, 0:1]), reads=list(rkeys) + ["mid"], writes=["jk", "cn"])
                kb.op("dve", lambda e: e.tensor_scalar(out=ge[:], in0=cn[:], scalar1=float(cap) - 0.5, scalar2=None, op0=ALU.is_gt), reads=["cn"], writes=["ge"])
                kb.op("dve", lambda e: e.tensor_tensor(out=d1[:], in0=mid[:], in1=lo[:], op=ALU.subtract), reads=["mid", "lo"], writes=["d1"])
                kb.op("dve", lambda e: e.tensor_tensor(out=d2[:], in0=hi[:], in1=mid[:], op=ALU.subtract), reads=["mid", "hi"], writes=["d2"])
                kb.op("dve", lambda e: e.scalar_tensor_tensor(out=lo[:], in0=d1[:], scalar=ge[:, 0:1], in1=lo[:], op0=ALU.mult, op1=ALU.add),
                      reads=["d1", "ge", "lo"], writes=["lo"])
                kb.op("dve", lambda e: e.scalar_tensor_tensor(out=hi[:], in0=d2[:], scalar=ge[:, 0:1], in1=mid[:], op0=ALU.mult, op1=ALU.add),
                      reads=["d2", "ge", "mid"], writes=["hi"])
            kb.op("dve", lambda e: e.tensor_copy(out=thr[:, col:col + 1], in_=lo[:]), reads=["lo"], writes=[("thr", col)])

        for b in range(1):
            for q in range(4):
                kb.dma("sp" if q % 2 == 0 else "pool", aall[:, q * 4096:(q + 1) * 4096], affM[b, :, q * 4096:(q + 1) * 4096], writes=[("aall", q)])
            bisect(aall[:, :], SEQ, 2 * SEQ // NE, [("aall", q) for q in range(4)], 2 * b)
            if need_ctx:
                kb.dma("sp", actx[:], affC[b, :, :], writes=["actx"])
                bisect(actx[:, :], NCTX, 2 * NCTX // NE, ["actx"], 2 * b + 1)
        segs = []
        for b in range(1):
            segs.append((b * NBT, b * NBT + SEQ, 2 * b))
            if need_ctx:
                segs.append((b * NBT + SEQ, (b + 1) * NBT, 2 * b + 1))
        for (s0, s1, col) in segs:
            for c0 in range(s0, s1, 4096):
                n = min(4096, s1 - c0)
                kb.dma("sp", aown[:, 0:n], affS[:, c0:c0 + n], writes=["aown"])
                kb.op("dve", lambda e: e.tensor_scalar(out=msk[:, 0:n], in0=aown[:, 0:n], scalar1=thr[:, col:col + 1], scalar2=None, op0=ALU.is_gt),
                      reads=["aown", ("thr", col)], writes=["msk"])
                kb.op("dve", lambda e: e.tensor_tensor(out=gs_t[:, 0:n], in0=msk[:, 0:n], in1=aown[:, 0:n], op=ALU.mult),
                      reads=["msk", "aown"], writes=["gs_t"])
                kb.dma("sp", gsel_d[:, c0:c0 + n], gs_t[:, 0:n], reads=["gs_t"], writes=["gsel_d"])
        cx.barrier()

    MAXT = PART_T * 128
    oh = cx.sb("oh", [NE, NE, 128])
    kb.op("pool", lambda e: e.memset(oh[:], 1.0), writes=["oh"])
    kb.op("pool", lambda e: e.affine_select(out=oh[:], in_=oh[:], pattern=[[-1, NE], [0, 128]], compare_op=ALU.is_equal, fill=0.0,
                                            base=0, channel_multiplier=1), reads=["oh"], writes=["oh"])
    gsel = cx.sb("gsel", [NE, MAXT])
    hsb = cx.sb("hsb", [128, 8, MAXT], BF16)
    yacc = cx.sb("yacc", [128, 8, MAXT])
    gbc = cx.sb("gbc", [128, MAXT])
    stg = cx.sb("stg", [128, 8, 256])
    stu = cx.sb("stu", [128, 8, 256])
    std = cx.sb("std", [128, 2, D])
    wgb = [cx.sb(f"wgb{i}", [128, 8, 256], BF16) for i in range(2)]
    wub = [cx.sb(f"wub{i}", [128, 8, 256], BF16) for i in range(2)]
    wdb = [cx.sb(f"wdb{i}", [128, 2, D], BF16) for i in range(2)]
    sg = cx.sb("sg", [128, 2, 512])
    ug = cx.sb("ug", [128, 2, 512])
    act = [cx.sb(f"act{i}", [128, 2, 512], BF16) for i in range(2)]
    pb = [cx.sb(f"pb{i}", [128, D], BF16) for i in range(2)]
    gps = cx.ps("gps", [128, 2, 512])
    ups = cx.ps("ups", [128, 2, 512])
    yps = [cx.ps(f"yps{i}", [128, 512]) for i in range(3)]
    pgb = cx.ps("pgb", [128, 512])
    h2T_v = h2T.rearrange("(c p) t -> p c t", p=128)
    cnt = {"w": 0, "y": 0, "a": 0}

    ntl_all = NALL // 128
    for t0 in range(0, ntl_all, PART_T):
        ntl = min(PART_T, ntl_all - t0)
        tok0, ntok = t0 * 128, ntl * 128
        blocks = [(b, min(512, ntok - b)) for b in range(0, ntok, 512)]
        kb.dma("sp", hsb[:, :, 0:ntok], h2T_v[:, :, tok0:tok0 + ntok], writes=["hsb"], slot=("hsb",))
        kb.dma("pool", gsel[:, 0:ntok], gsel_d[:, tok0:tok0 + ntok], reads=["gsel_d"], writes=["gsel"], slot=("gsel",))
        kb.op("pool", lambda e: e.memset(yacc[:], 0.0), writes=[("yacc", dc) for dc in range(8)])
        for ex in range(NEC):
            for (b0, n) in blocks:
                kb.op("pe", lambda e: e.matmul(pgb[:, 0:n], lhsT=oh[:, ex, :], rhs=gsel[:, b0:b0 + n], start=True, stop=True),
                      reads=["oh", "gsel"], writes=["pgb"])
                kb.op("act", lambda e: e.copy(out=gbc[:, b0:b0 + n], in_=pgb[:, 0:n]), reads=["pgb"], writes=["gbc"])
            for jp in range(DFF // 256):
                ws = cnt["w"] % 2
                cnt["w"] += 1
                f0 = jp * 256
                kb.dma("sp", stg[:], wg[ex].rearrange("(c p) f -> p c f", p=128)[:, :, f0:f0 + 256], writes=["stg"], slot=("stg",))
                kb.dma("sp", stu[:], wu[ex].rearrange("(c p) f -> p c f", p=128)[:, :, f0:f0 + 256], writes=["stu"], slot=("stu",))
                kb.dma("pool", std[:], wd[ex, f0:f0 + 256, :].rearrange("(j p) d -> p j d", p=128), writes=["std"], slot=("std",))
                kb.op("pool", lambda e: e.tensor_copy(out=wgb[ws][:], in_=stg[:]), reads=["stg"], writes=[("wgb", ws)])
                kb.op("pool", lambda e: e.tensor_copy(out=wub[ws][:], in_=stu[:]), reads=["stu"], writes=[("wub", ws)])
                kb.op("pool", lambda e: e.tensor_copy(out=wdb[ws][:], in_=std[:]), reads=["std"], writes=[("wdb", ws)])
                for (b0, n) in blocks:
                    ai = cnt["a"] % 2
                    cnt["a"] += 1
                    for jj in range(2):
                        for c in range(8):
                            kb.op("pe", lambda e: e.matmul(gps[:, jj, 0:n], lhsT=wgb[ws][:, c, jj * 128:(jj + 1) * 128], rhs=hsb[:, c, b0:b0 + n],
                                                           start=(c == 0), stop=(c == 7)),
                                  reads=[("wgb", ws), "hsb"], writes=[("gps", jj)])
                        for c in range(8):
                            kb.op("pe", lambda e: e.matmul(ups[:, jj, 0:n], lhsT=wub[ws][:, c, jj * 128:(jj + 1) * 128], rhs=hsb[:, c, b0:b0 + n],
                                                           start=(c == 0), stop=(c == 7)),
                                  reads=[("wub", ws), "hsb"], writes=[("ups", jj)])
                    for jj in range(2):
                        kb.op("act", lambda e: e.activation(out=sg[:, jj, 0:n], in_=gps[:, jj, 0:n], func=AF.Silu), reads=[("gps", jj)], writes=[("sg", jj)])
                        kb.op("dve", lambda e: e.tensor_tensor(out=ug[:, jj, 0:n], in0=ups[:, jj, 0:n], in1=gbc[:, b0:b0 + n], op=ALU.mult),
                              reads=[("ups", jj), "gbc"], writes=[("ug", jj)])
                        kb.op("pool", lambda e: e.tensor_tensor(out=act[ai][:, jj, 0:n], in0=sg[:, jj, 0:n], in1=ug[:, jj, 0:n], op=ALU.mult),
                              reads=[("sg", jj), ("ug", jj)], writes=[("act", ai, jj)])
                    for dc in range(8):
                        yi = cnt["y"] % 3
                        cnt["y"] += 1
                        for jj in range(2):
                            kb.op("pe", lambda e: e.matmul(yps[yi][:, 0:n], lhsT=wdb[ws][:, jj, dc * 128:(dc + 1) * 128], rhs=act[ai][:, jj, 0:n],
                                                           start=(jj == 0), stop=(jj == 1)),
                                  reads=[("wdb", ws), ("act", ai, jj)], writes=[("yps", yi)])
                        kb.op("dve", lambda e: e.tensor_tensor(out=yacc[:, dc, b0:b0 + n], in0=yps[yi][:, 0:n], in1=yacc[:, dc, b0:b0 + n], op=ALU.add),
                              reads=[("yps", yi), ("yacc", dc)], writes=[("yacc", dc)])
        for tt in range(ntl):
            t = t0 + tt
            s = tt % 2
            for dc in range(8):
                kb.op("pe", lambda e: e.transpose(out=gps[:, dc // 4, (dc % 4) * 128:(dc % 4 + 1) * 128], in_=yacc[:, dc, tt * 128:(tt + 1) * 128],
                                                  identity=identf[:]),
                      reads=[("yacc", dc), "ident_f"], writes=[("gps", dc // 4)])
            for hb_ in range(2):
                kb.op("act", lambda e: e.copy(out=pb[s][:, hb_ * 512:(hb_ + 1) * 512], in_=gps[:, hb_, :]), reads=[("gps", hb_)], writes=[("pb", s, hb_)])
            kb.dma("sp", part_o[t * 128:(t + 1) * 128, :], pb[s][:], reads=[("pb", s, 0), ("pb", s, 1)], writes=["part_d"], slot=("pbo", s))
    return cx.finish()


def build_C(l):
    need_ctx = (l == 0)
    NQ = NTOK if need_ctx else NT
    cx = Ctx()
    nc, kb = cx.nc, cx.kb
    x1 = cx.din("x1", [NQ, D])
    parts = cx.din("parts", [NE // NEC, NQ, D], BF16)
    modv = cx.din("modv", [2, 6 * D])
    x2_o = cx.dout("x2", [NQ, D])
    G2 = cx.sb("G2", [128, 2, D])
    for m in range(2):
        kb.dma("sp", G2[:, m, :], modv[m:m + 1, 5 * D:6 * D].broadcast_to([128, D]), writes=[("G2", m)])
    xt = [cx.sb(f"xt{i}", [128, D]) for i in range(2)]
    pt = [cx.sb(f"pt{i}", [128, NE // NEC, D], BF16) for i in range(2)]
    acc = cx.sb("acc", [128, D])
    ot = [cx.sb(f"ot{i}", [128, D]) for i in range(2)]
    for t in range(NQ // 128):
        m = 0 if t < NT // 128 else 1
        s = t % 2
        dq = "sp" if s == 0 else "pool"
        kb.dma(dq, xt[s][:], x1[t * 128:(t + 1) * 128, :], writes=[("xt", s)], slot=("xtC", s))
        kb.dma(dq, pt[s][:], parts[:, t * 128:(t + 1) * 128, :].rearrange("k p d -> p k d"), writes=[("pt", s)], slot=("ptC", s))
        kb.op("dve", lambda e: e.tensor_tensor(out=acc[:], in0=pt[s][:, 0, :], in1=pt[s][:, 1, :], op=ALU.add), reads=[("pt", s)], writes=["acc"])
        for k in range(2, NE // NEC):
            kb.op("dve", lambda e: e.tensor_tensor(out=acc[:], in0=acc[:], in1=pt[s][:, k, :], op=ALU.add), reads=["acc", ("pt", s)], writes=["acc"])
        kb.op("dve", lambda e: e.tensor_tensor(out=acc[:], in0=acc[:], in1=G2[:, m, :], op=ALU.mult), reads=["acc", ("G2", m)], writes=["acc"])
        kb.op("pool", lambda e: e.tensor_tensor(out=ot[s][:], in0=acc[:], in1=xt[s][:], op=ALU.add), reads=["acc", ("xt", s)], writes=[("ot", s)])
        kb.dma(dq, x2_o[t * 128:(t + 1) * 128, :], ot[s][:], reads=[("ot", s)], writes=["x2_d"], slot=("x2C", s))
    return cx.finish()


def run_E2(inp, l, resA):
    nc = _get_nc("E2", build_E2, l)
    need_ctx = (l == 0)
    affs = []
    h2s = []
    for b in range(2):
        grp = [resA[4 * b + rr] for rr in range(4)]
        a = [g["aff"][0:NT] for g in grp]
        h = [g["h2T"][:, 0:NT] for g in grp]
        if need_ctx:
            a.append(grp[0]["aff"][NT:NTOK])
            h.append(grp[0]["h2T"][:, NT:NTOK])
        affs.append(np.concatenate(a, axis=0))
        h2s.append(np.ascontiguousarray(np.concatenate(h, axis=1)))
    in_maps = []
    for c in range(NCORE):
        b, k = c // 4, c % 4
        perm = list(range(NEC * k, NEC * (k + 1))) + [e for e in range(NE) if not (NEC * k <= e < NEC * (k + 1))]
        affT = affs[b].T[perm]
        affM = affT[None, :, 0:SEQ]
        affC = affT[None, :, SEQ:SEQ + NCTX] if need_ctx else np.zeros((1, NE, NCTX), np.float32)
        in_maps.append({
            "h2T": h2s[b], "affM": np.ascontiguousarray(affM), "affC": np.ascontiguousarray(affC),
            "affS": np.ascontiguousarray(affT),
            "wg": np.ascontiguousarray(inp["w_gate"][l][NEC * k:NEC * (k + 1)]),
            "wu": np.ascontiguousarray(inp["w_up"][l][NEC * k:NEC * (k + 1)]),
            "wd": np.ascontiguousarray(inp["w_down"][l][NEC * k:NEC * (k + 1)]),
        })
    res = run_bass_kernel_spmd(nc, in_maps, core_ids=list(range(NCORE)))
    return res.results


def run_C(l, resA, resE, modvs):
    nc = _get_nc("C", build_C, l)
    need_ctx = (l == 0)
    NBT = SEQ + (NCTX if need_ctx else 0)
    in_maps = []
    for c in range(NCORE):
        b, r = c // 4, c % 4
        ps = []
        for k in range(NE // NEC):
            p = resE[4 * b + k]["part"]
            rows = [p[r * NT:(r + 1) * NT]]
            if need_ctx:
                rows.append(p[SEQ:SEQ + NCTX])
            ps.append(np.concatenate(rows, axis=0))
        in_maps.append({"x1": resA[c]["x1"], "parts": np.ascontiguousarray(np.stack(ps, axis=0)), "modv": modvs[c]})
    res = run_bass_kernel_spmd(nc, in_maps, core_ids=list(range(NCORE)))
    return res.results


def kernel(**inp):
    inp = {k: np.asarray(v) for k, v in inp.items()}
    x_cur = np.asarray(inp["x"], np.float32)
    xc_cur = np.asarray(inp["ctx"], np.float32)
    rope_tab = _rope_tables()
    for l in range(2):
        resP = run_P(inp, l, x_cur, xc_cur, rope_tab)
        modvs = [resP[c]["modv"] for c in range(NCORE)]
        resA, _ = run_A(inp, l, resP, x_cur, xc_cur)
        del resP
        resE = run_E2(inp, l, resA)
        resC = run_C(l, resA, resE, modvs)
        del resA, resE
        x_cur = np.stack([np.concatenate([resC[4 * b + r]["x2"][0:NT] for r in range(4)], axis=0) for b in range(2)], axis=0)
        if l == 0:
            xc_cur = np.stack([resC[4 * b]["x2"][NT:NTOK] for b in range(2)], axis=0)
    return np.ascontiguousarray(x_cur.astype(np.float32))
```
